# Optimizing a Trainium2 kernel written in Bass

```python
import jax, jax.numpy as jnp
from jax import lax
import numpy as np

D_MODEL = 1024
BATCH = 4
SEQ = 8192
DEPTH = 1

PLE_DIM = 256
HEAD_DIM = 64
N_ATTN_HEADS = 8
ATTN_WIDTH = N_ATTN_HEADS * HEAD_DIM
CONV_WIDTH = D_MODEL - ATTN_WIDTH
N_CONV_GROUPS = 8
CONV_GROUP_DIM = CONV_WIDTH // N_CONV_GROUPS
MIX_WIDTH = ATTN_WIDTH + CONV_WIDTH
CONV_K = 3
DILATED_GROUPS = ((128, 1), (512, 4), (2048, 16))
ATTN_BLOCK = 128
ROPE_THETA = 500000.0
ROPE_DIM = HEAD_DIM // 4
N_GROUPS = 4
EXPERTS_PER_GROUP = 4
N_EXPERTS = N_GROUPS * EXPERTS_PER_GROUP
TOP_K = 2
D_FF_EXPERT = 512
EPS = 1e-6

kernel_name = "hymba_conv_dilated_attn_hmoe_ple"


def rmsnorm(x, g):
    x32 = x.astype(jnp.float32)
    y = x32 * lax.rsqrt(jnp.mean(x32 * x32, axis=-1, keepdims=True) + EPS)
    return (y * g.astype(jnp.float32)).astype(x.dtype)


def partial_rope(x, pos):
    half = ROPE_DIM // 2
    inv = ROPE_THETA ** (-jnp.arange(0, ROPE_DIM, 2, dtype=jnp.float32) / ROPE_DIM)
    ang = pos.astype(jnp.float32)[:, None] * inv[None, :]
    cos = jnp.cos(ang)[None, :, None, :]
    sin = jnp.sin(ang)[None, :, None, :]
    x32 = x.astype(jnp.float32)
    x1 = x32[..., :half]
    x2 = x32[..., half:ROPE_DIM]
    rot = jnp.concatenate([x1 * cos - x2 * sin, x2 * cos + x1 * sin], axis=-1).astype(x.dtype)
    return jnp.concatenate([rot, x[..., ROPE_DIM:]], axis=-1)


def dilated_window_branch(q, k, v, window, dilation):
    B, S, H, Dh = q.shape
    n_back = window // dilation
    L = S // dilation
    nb = -(-L // ATTN_BLOCK)
    Lp = nb * ATTN_BLOCK

    def split(t):
        t = t.reshape(B, L, dilation, H, Dh)
        t = jnp.pad(t, ((0, 0), (0, Lp - L), (0, 0), (0, 0), (0, 0)))
        return t.reshape(B, nb, ATTN_BLOCK, dilation, H, Dh)

    def with_prev(t):
        prev = jnp.pad(t, ((0, 0), (1, 0), (0, 0), (0, 0), (0, 0), (0, 0)))[:, :-1]
        return jnp.concatenate([prev, t], axis=2)

    qb = split(q)
    kk = with_prev(split(k))
    vv = with_prev(split(v))
    s = jnp.einsum('bnqrhd,bnkrhd->bnrhqk', qb, kk,
                   preferred_element_type=jnp.float32)
    qi = jnp.arange(ATTN_BLOCK)[:, None]
    ki = jnp.arange(2 * ATTN_BLOCK)[None, :]
    dist = qi + ATTN_BLOCK - ki
    band = (dist >= 0) & (dist <= n_back)
    exists = (jnp.arange(nb)[:, None, None] > 0) | (ki[None] >= ATTN_BLOCK)
    valid = band[None] & exists
    s = jnp.where(valid[None, :, None, None], s, -jnp.inf)
    m = jnp.max(s, axis=-1)
    e = jnp.exp(s - m[..., None])
    den = jnp.sum(e, axis=-1)
    num = jnp.einsum('bnrhqk,bnkrhd->bnqrhd', e, vv.astype(jnp.float32))
    num = num.reshape(B, Lp, dilation, H, Dh)[:, :L].reshape(B, S, H, Dh)

    def unblock(t):
        t = t.transpose(0, 1, 4, 2, 3).reshape(B, Lp, dilation, H)
        return t[:, :L].reshape(B, S, H)

    return num, unblock(m), unblock(den)


def dilated_mixture_attention(q, k, v):
    parts = [dilated_window_branch(q, k, v, w, d) for (w, d) in DILATED_GROUPS]
    m_all = jnp.max(jnp.stack([pm for (_, pm, _) in parts], axis=0), axis=0)
    num = 0.0
    den = 0.0
    for (pn, pm, pd) in parts:
        w = jnp.exp(pm - m_all)
        num = num + w[..., None] * pn
        den = den + w * pd
    return num / den[..., None]


def short_gated_conv(b_gate, c_gate, hx, w):
    u = c_gate * hx
    y = lax.conv_general_dilated(
        u, w[:, None, :].astype(u.dtype), window_strides=(1,),
        padding=((CONV_K - 1, 0),), dimension_numbers=('NWC', 'WIO', 'NWC'),
        feature_group_count=u.shape[-1])
    return b_gate * y


def hierarchical_moe(x, w_rg, b_rg, w_re, b_re, w1, w3, w2):
    B, S, _ = x.shape
    lg = jnp.einsum('bsd,dg->bsg', x, w_rg, preferred_element_type=jnp.float32) + b_rg.astype(jnp.float32)
    pg = jax.nn.softmax(lg, axis=-1)
    g_idx = jnp.argmax(lg, axis=-1)
    pg_top = jnp.take_along_axis(pg, g_idx[..., None], axis=-1)
    le = jnp.einsum('bsd,de->bse', x, w_re, preferred_element_type=jnp.float32) + b_re.astype(jnp.float32)
    le = le.reshape(B, S, N_GROUPS, EXPERTS_PER_GROUP)
    le_sel = jnp.take_along_axis(le, g_idx[..., None, None], axis=-2)[..., 0, :]
    pe = jax.nn.softmax(le_sel, axis=-1)
    tv, ti = lax.top_k(pe, TOP_K)
    tv = tv / jnp.sum(tv, axis=-1, keepdims=True)
    within = jnp.sum(jax.nn.one_hot(ti, EXPERTS_PER_GROUP, dtype=jnp.float32) * tv[..., None], axis=-2)
    gates = (pg_top[..., None]
             * jax.nn.one_hot(g_idx, N_GROUPS, dtype=jnp.float32)[..., None]
             * within[..., None, :]).reshape(B, S, N_EXPERTS).astype(x.dtype)
    y = jnp.zeros_like(x)
    for e in range(N_EXPERTS):
        hdn = jax.nn.silu(x @ w1[e]) * (x @ w3[e])
        y = y + gates[..., e:e + 1] * (hdn @ w2[e])
    return y


def setup_inputs(seed: int = 0) -> dict:
    key = jax.random.key(seed)
    ks = jax.random.split(key, 24)
    f32 = jnp.float32

    def nrm(k, shape, fan_in):
        return jax.random.normal(k, shape, f32) * (fan_in ** -0.5)

    def gain(k, shape):
        return 1.0 + 0.05 * jax.random.normal(k, shape, f32)

    n_in = 3 * ATTN_WIDTH + 3 * CONV_WIDTH
    return {
        "x": jax.random.normal(ks[0], (BATCH, SEQ, D_MODEL), f32),
        "p": jax.random.normal(ks[1], (DEPTH, BATCH, SEQ, PLE_DIM), f32),
        "g_mix": gain(ks[2], (DEPTH, D_MODEL)),
        "w_in": nrm(ks[3], (DEPTH, D_MODEL, n_in), D_MODEL),
        "q_norm": gain(ks[4], (DEPTH, HEAD_DIM)),
        "k_norm": gain(ks[5], (DEPTH, HEAD_DIM)),
        "conv_w": nrm(ks[6], (DEPTH, CONV_K, CONV_WIDTH), CONV_K),
        "g_attn_out": gain(ks[7], (DEPTH, ATTN_WIDTH)),
        "g_conv_out": gain(ks[8], (DEPTH, CONV_WIDTH)),
        "w_out": nrm(ks[9], (DEPTH, MIX_WIDTH, D_MODEL), MIX_WIDTH),
        "g_ffn": gain(ks[10], (DEPTH, D_MODEL)),
        "w_router_group": nrm(ks[11], (DEPTH, D_MODEL, N_GROUPS), D_MODEL),
        "b_router_group": 0.01 * jax.random.normal(ks[12], (DEPTH, N_GROUPS), f32),
        "w_router_expert": nrm(ks[13], (DEPTH, D_MODEL, N_EXPERTS), D_MODEL),
        "b_router_expert": 0.01 * jax.random.normal(ks[14], (DEPTH, N_EXPERTS), f32),
        "w1": nrm(ks[15], (DEPTH, N_EXPERTS, D_MODEL, D_FF_EXPERT), D_MODEL),
        "w3": nrm(ks[16], (DEPTH, N_EXPERTS, D_MODEL, D_FF_EXPERT), D_MODEL),
        "w2": nrm(ks[17], (DEPTH, N_EXPERTS, D_FF_EXPERT, D_MODEL), D_FF_EXPERT),
        "g_ple": gain(ks[18], (DEPTH, D_MODEL)),
        "w_ple_gate": nrm(ks[19], (DEPTH, D_MODEL, D_MODEL), D_MODEL),
        "w_ple_proj": nrm(ks[20], (DEPTH, PLE_DIM, D_MODEL), PLE_DIM),
        "g_ple_post": gain(ks[21], (DEPTH, D_MODEL)),
    }


def reference(x, p, g_mix, w_in, q_norm, k_norm, conv_w, g_attn_out, g_conv_out, w_out,
              g_ffn, w_router_group, b_router_group, w_router_expert, b_router_expert,
              w1, w3, w2, g_ple, w_ple_gate, w_ple_proj, g_ple_post):
    B, S, _ = x.shape
    pos = jnp.arange(S)
    A, C = ATTN_WIDTH, CONV_WIDTH
    h = x
    for i in range(DEPTH):
        xn = rmsnorm(h, g_mix[i])
        z = xn @ w_in[i]
        q, k, v, cb, cc, ch = jnp.split(z, [A, 2 * A, 3 * A, 3 * A + C, 3 * A + 2 * C], axis=-1)
        q = q.reshape(B, S, N_ATTN_HEADS, HEAD_DIM)
        k = k.reshape(B, S, N_ATTN_HEADS, HEAD_DIM)
        v = v.reshape(B, S, N_ATTN_HEADS, HEAD_DIM)
        q = partial_rope(rmsnorm(q, q_norm[i]), pos) * (HEAD_DIM ** -0.5)
        k = partial_rope(rmsnorm(k, k_norm[i]), pos)
        attn = dilated_mixture_attention(q, k, v).astype(h.dtype)
        attn = rmsnorm(attn, g_attn_out[i].reshape(N_ATTN_HEADS, HEAD_DIM)).reshape(B, S, A)
        conv = short_gated_conv(cb, cc, ch, conv_w[i])
        conv = rmsnorm(conv.reshape(B, S, N_CONV_GROUPS, CONV_GROUP_DIM),
                       g_conv_out[i].reshape(N_CONV_GROUPS, CONV_GROUP_DIM)).reshape(B, S, C)
        h = h + jnp.concatenate([attn, conv], axis=-1) @ w_out[i]
        h = h + hierarchical_moe(rmsnorm(h, g_ffn[i]), w_router_group[i], b_router_group[i],
                                 w_router_expert[i], b_router_expert[i], w1[i], w3[i], w2[i])
        gate = jax.nn.sigmoid(rmsnorm(h, g_ple[i]) @ w_ple_gate[i])
        h = h + gate * rmsnorm(p[i] @ w_ple_proj[i], g_ple_post[i])
    return h
```

```python
import contextlib
import numpy as np
import concourse.bass as bass
import concourse.mybir as mybir
from concourse.bass_utils import run_bass_kernel_spmd

F32 = mybir.dt.float32
BF16 = mybir.dt.bfloat16
I32 = mybir.dt.int32
ALU = mybir.AluOpType
AF = mybir.ActivationFunctionType
AX = mybir.AxisListType

NTOK = 4096
NHALO = 2048
NALL = NTOK + NHALO
TS = 512
NT = 32
EPS = 1e-6
P1_RANGE = range(48)
BRANCHES = (1, 4, 16)
P2_STOP = 4
MASK_ENG = 'dve'


SEG_T = []


class Sched:
    ENGS = ("pe", "act", "dve", "pool", "sp")
    DEF_COST = {"pe": 0.25, "act": 0.7, "dve": 0.55, "pool": 1.3, "sp": 0.15}
    DMA_LAT = 3.0
    WINDOW = 96

    def __init__(self, nc):
        self.nc = nc
        self.ops = []
        self.last_w = {}
        self.readers = {}
        self.dma_eng = {}
        self.seg = 0

    def op(self, eng, fn, r=(), w=(), dma=None, c=None):
        oid = len(self.ops)
        deps = set()
        for k in r:
            if k in self.last_w:
                deps.add(self.last_w[k])
        for k in w:
            if k in self.last_w:
                deps.add(self.last_w[k])
            for rd in self.readers.get(k, ()):
                deps.add(rd)
        for k in r:
            self.readers.setdefault(k, []).append(oid)
        for k in w:
            self.last_w[k] = oid
            self.readers[k] = []
        if dma is not None:
            assert self.dma_eng.setdefault(dma, eng) == eng
        self.ops.append(dict(eng=eng, fn=fn, deps=deps, dma=dma, sig=False, idx=None, seg=self.seg, c=c))
        return oid

    def barrier(self):
        self.seg += 1

    def schedule(self):
        ops = self.ops
        n = len(ops)
        ENGS = self.ENGS
        finish = [0.0] * n
        eng_free = {e: 0.0 for e in ENGS}
        order = {e: [] for e in ENGS}
        last_eng = {}
        last_dma = {}
        byseg = [[] for _ in range(self.seg + 1)]
        for i, o in enumerate(ops):
            byseg[o["seg"]].append(i)
        tseg = 0.0
        W = self.WINDOW
        for s, ids in enumerate(byseg):
            if not ids:
                continue
            bar = set(last_eng.values()) | set(last_dma.values())
            if s > 0:
                for i in ids:
                    ops[i]["deps"] |= bar
            idset = set(ids)
            users = {i: [] for i in ids}
            nun = {}
            ready = {}
            for i in ids:
                k = 0
                for d in ops[i]["deps"]:
                    if d in idset:
                        users[d].append(i)
                        k += 1
                nun[i] = k
                ready[i] = tseg
            pending = {e: [i for i in ids if ops[i]["eng"] == e] for e in ENGS}
            for e in ENGS:
                eng_free[e] = max(eng_free[e], tseg)
            remaining = len(ids)
            while remaining:
                best = None
                for e in ENGS:
                    ef = eng_free[e]
                    cand = None
                    cnt = 0
                    for i in pending[e]:
                        cnt += 1
                        if cnt > W:
                            break
                        if nun[i]:
                            continue
                        rt = ready[i]
                        if rt <= ef:
                            cand = (ef, i)
                            break
                        if cand is None or rt < cand[0]:
                            cand = (rt, i)
                    if cand is not None and (best is None or cand[0] < best[0]):
                        best = (cand[0], e, cand[1])
                t0, e, i = best
                o = ops[i]
                if o["dma"] is not None:
                    issue = 1.1 if e == "pool" else 0.15
                    fin = t0 + issue + (o["c"] if o["c"] is not None else self.DMA_LAT)
                    last_dma[o["dma"]] = i
                else:
                    issue = o["c"] if o["c"] is not None else self.DEF_COST[e]
                    fin = t0 + issue + 0.1
                    last_eng[e] = i
                eng_free[e] = t0 + issue
                finish[i] = fin
                pending[e].remove(i)
                order[e].append(i)
                remaining -= 1
                for u in users[i]:
                    nun[u] -= 1
                    if ready[u] < fin:
                        ready[u] = fin
            tseg = max(finish[i] for i in ids)
            tseg = max([tseg] + list(eng_free.values()))
            SEG_T.append(round(tseg, 1))
        self.est_total = tseg
        return order

    def emit(self, final_wait_ops=()):
        nc = self.nc
        ops = self.ops
        order = self.schedule()
        dcnt = {}
        for e in self.ENGS:
            for i in order[e]:
                o = ops[i]
                if o["dma"] is not None:
                    c = dcnt.get(o["dma"], 0) + 1
                    dcnt[o["dma"]] = c
                    o["dcount"] = c
        for o in ops:
            for d in o["deps"]:
                p = ops[d]
                if p["dma"] is None:
                    if p["eng"] == "pe" and o["eng"] == "pe" and o["dma"] is None:
                        continue
                    p["sig"] = True
        for d in final_wait_ops:
            if ops[d]["dma"] is None:
                ops[d]["sig"] = True
        for e in self.ENGS:
            k = 0
            for i in order[e]:
                o = ops[i]
                if o["dma"] is None and o["sig"]:
                    k += 1
                    o["idx"] = k
        with contextlib.ExitStack() as es:
            esem = {e: es.enter_context(nc.semaphore("S_" + e)) for e in self.ENGS}
            dsem = {}
            for i, k in enumerate(dcnt):
                dsem[k] = es.enter_context(nc.semaphore("D%d" % i))
            block = es.enter_context(nc.Block())

            def run(engname, engobj):
                waited = {}
                for oi in order[engname]:
                    o = ops[oi]
                    need = {}
                    for d in sorted(o["deps"]):
                        p = ops[d]
                        if p["dma"] is not None:
                            s, v = dsem[p["dma"]], 16 * p["dcount"]
                        else:
                            if p["idx"] is None:
                                continue
                            if p["eng"] == "pe" and engname == "pe" and o["dma"] is None:
                                continue
                            s, v = esem[p["eng"]], p["idx"]
                        key = id(s)
                        if key not in need or need[key][1] < v:
                            need[key] = (s, v)
                    for key, (s, v) in need.items():
                        if waited.get(key, 0) >= v:
                            continue
                        waited[key] = v
                        engobj.wait_ge(s, v)
                    ins = o["fn"](engobj)
                    if o["dma"] is not None:
                        ins.then_inc(dsem[o["dma"]], 16)
                    elif o["sig"]:
                        ins.then_inc(esem[engname], 1)
                if engname == "sp":
                    for d in final_wait_ops:
                        p = ops[d]
                        if p["dma"] is not None:
                            engobj.wait_ge(dsem[p["dma"]], 16 * p["dcount"])
                        else:
                            engobj.wait_ge(esem[p["eng"]], p["idx"])

            @block.tensor
            def _(e):
                run("pe", e)

            @block.scalar
            def _(e):
                run("act", e)

            @block.vector
            def _(e):
                run("dve", e)

            @block.gpsimd
            def _(e):
                run("pool", e)

            @block.sync
            def _(e):
                run("sp", e)


def RS(ap, *dims):
    names = "abcdef"[:len(dims)]
    kw = {n: d for n, d in zip(names[:-1], dims[:-1])}
    return ap.rearrange("p (%s) -> p %s" % (" ".join(names), " ".join(names)), **kw)


class Arena:
    def __init__(self, t, nwords):
        self.t = t
        self.n = nwords
        self.n0 = nwords
        self.off = 0
        self.hw = 0

    def alloc(self, dims, dtype=F32):
        n = int(np.prod(dims))
        sz = {F32: 4, BF16: 2, I32: 4}[dtype]
        words = (n * sz + 3) // 4
        words = (words + 7) // 8 * 8
        assert self.off + words <= self.n, ("arena overflow", self.off, words, self.n)
        ap = self.t[:, self.off:self.off + words]
        self.off += words
        self.hw = max(self.hw, self.off)
        if dtype != F32:
            ap = ap.bitcast(dtype)
        ap = ap[:, 0:n]
        if len(dims) > 1:
            ap = RS(ap, *dims)
        return ap

    def alloc_top(self, dims, dtype=F32):
        n = int(np.prod(dims))
        sz = {F32: 4, BF16: 2, I32: 4}[dtype]
        words = ((n * sz + 3) // 4 + 7) // 8 * 8
        self.n -= words
        assert self.off <= self.n, ("arena overflow (top)", self.off, words, self.n)
        ap = self.t[:, self.n:self.n + words]
        if dtype != F32:
            ap = ap.bitcast(dtype)
        ap = ap[:, 0:n]
        if len(dims) > 1:
            ap = RS(ap, *dims)
        return ap

    def free_top(self):
        self.n = self.n0

    def mark(self):
        return self.off

    def reset(self, m):
        self.off = m


def build_nc(upto=99, dbg=False):
    nc = bass.Bass("TRN2", target_bir_lowering=False)
    D = lambda n, s, dt=F32: nc.dram_tensor(n, s, dt, kind="ExternalInput").ap()
    xin = D("xin", [NALL, 1024])
    pin = D("pin", [NTOK, 256])
    exd = D("ex", [128, 48])
    cosd = D("cos", [128, 48 * 8])
    sind = D("sin", [128, 48 * 8])
    gmixd = D("gmix", [128, 8])
    gqd = D("gq_b", [128, 512])
    gkd = D("gk_b", [128, 512])
    cwd = D("cw", [128, 12])
    gaod = D("gout_pc", [128, 8])
    gffnbd = D("gffn_b", [128, 1024])
    gffnpc = D("gffn_pc", [128, 8])
    gplepc = D("gple_pc", [128, 8])
    gpostbd = D("gpost_b", [128, 1024])
    rbiasd = D("rbias_b", [128, 20])
    w_in = D("w_in", [1024, 3072])
    w_out = D("w_out", [1024, 1024])
    w_r = D("w_r", [1024, 20])
    w1 = D("w1", [16 * 128, 8 * 512])
    w3 = D("w3", [16 * 128, 8 * 512])
    w2 = D("w2", [16 * 128, 4 * 1024])
    w_pg = D("w_pg", [1024, 1024])
    w_pp = D("w_pp", [256, 1024])
    identd = D("ident", [128, 128])
    mask4d = D("mask4", [128, 512])
    blk64d = D("blk64", [128, 128])
    wredad = D("wreda", [128, 128])
    wredbd = D("wredb", [128, 128])
    triud = D("triu", [128, 128])
    thrd = D("thr", [128, 17])
    tstartd = D("tstart", [128, NT])
    pidxd = D("pidx", [128, 1])
    out = nc.dram_tensor("out", [NTOK, 1024], F32, kind="ExternalOutput").ap()

    I = lambda n, s, dt: nc.dram_tensor(n, s, dt, kind="Internal").ap()
    QT = I("QT", [4, 128, NTOK], BF16)
    KT = I("KT", [4, 128, NALL], BF16)
    Vs2 = I("Vs2", [4, NALL, 192], BF16)
    Vs1 = I("Vs1", [4, 128, 48 * 192], BF16)
    CT = I("CT", [4, 128, NTOK], BF16)
    AT = I("AT", [4, 128, NTOK], BF16)
    Hd = I("Hd", [NTOK, 1024], F32)
    Xs = I("Xs", [NT * TS, 1024], BF16)
    Ys = I("Ys", [NT * TS, 1024], F32)
    dbgo = {}
    if dbg:
        O = lambda n, s, dt=F32: nc.dram_tensor(n, s, dt, kind="ExternalOutput").ap()
        dbgo["gates"] = O("d_gates", [128, 64])
        dbgo["pos"] = O("d_pos", [128, 64], I32)
        dbgo["widx"] = O("d_widx", [128, NT], I32)
        dbgo["lg"] = O("d_lg", [128, 32 * 20])

    NW = 51 * 1024
    with contextlib.ExitStack() as es:
        arena_t = es.enter_context(nc.sbuf_tensor("arena", [128, NW], F32))
        banks = [es.enter_context(nc.psum_tensor("bank%d" % i, [128, 512], F32)) for i in range(8)]
        A = Arena(arena_t, NW)
        S = Sched(nc)
        fin = []
        pw_keys = {}

        def PB(i):
            return banks[i][:].bitcast(BF16)

        def ld(dst, src, key, eng="sp"):
            return S.op(eng, lambda e: e.dma_start(out=dst, in_=src), w=[key], dma="ldc")

        stg = A.alloc([4096])
        stg_b = A.alloc([4096])
        identf = A.alloc([128]); ld(identf, identd[:, :], "identf")
        identb = A.alloc([128], BF16)
        epst = A.alloc([1])
        S.op("pool", lambda e: e.memset(epst, EPS), w=["epst"])

        def const_bf(src_d, n, key):
            t = A.alloc([n], BF16)
            S.op("sp", lambda e: e.dma_start(out=stg[:, 0:n], in_=src_d), w=["stg0"], dma=("ld", "stg0"))
            S.op("dve", lambda e: e.tensor_copy(out=t, in_=stg[:, 0:n]), r=["stg0"], w=[key])
            return t

        mask4 = const_bf(mask4d[:, :], 512, "mask4")
        blk64 = const_bf(blk64d[:, :], 128, "blk64")
        wreda = const_bf(wredad[:, :], 128, "wreda")
        wredb = const_bf(wredbd[:, :], 128, "wredb")
        triu = const_bf(triud[:, :], 128, "triu")
        onesb = A.alloc([128], BF16)
        S.op("pool", lambda e: e.memset(onesb, 1.0), w=["onesb"])

        def const_f(src_d, dims, key):
            t = A.alloc(dims)
            n = int(np.prod(dims))
            flat = t if len(dims) == 1 else t.rearrange("p a b -> p (a b)")
            ld(flat, src_d, key)
            return t

        ext = const_f(exd[:, :], [48], "ex")
        cost = const_f(cosd[:, :], [48, 8], "cos")
        sint = const_f(sind[:, :], [48, 8], "sin")
        gmix = const_f(gmixd[:, :], [8], "gmix")
        gao = const_f(gaod[:, :], [8], "gao")
        gffnpc_t = const_f(gffnpc[:, :], [8], "gffnpc")
        gplepc_t = const_f(gplepc[:, :], [8], "gplepc")
        cw = const_f(cwd[:, :], [4, 3], "cw")
        rbias = const_f(rbiasd[:, :], [20], "rbias")
        thr = const_f(thrd[:, :], [17], "thr")
        tstart = const_f(tstartd[:, :], [NT], "tstart")
        pidx = const_f(pidxd[:, :], [1], "pidx")
        lgall = A.alloc([32, 20])
        gates = A.alloc([2, 32])
        pos_i = A.alloc([2, 32], I32)
        widx_i = A.alloc([NT], I32)
        base_mark = A.mark()
        S.barrier()
        S.op("dve", lambda e: e.tensor_copy(out=identb, in_=identf), r=["identf"], w=["identb"])

        stg2 = [stg, stg_b]
        stg_n = [0]

        def prep_weight(dst_bf, src_rows_ap, nchunk, ncols, gain_pc, key, engs=("dve", "act")):
            si = stg_n[0] % 2
            stg_n[0] += 1
            sk = "stg%d" % si
            sv = RS(stg2[si][:, 0:nchunk * ncols], nchunk, ncols)
            S.op("sp", lambda e: e.dma_start(out=sv, in_=src_rows_ap), w=[sk], dma=("ld", sk), c=14.0)
            for c in range(nchunk):
                en = engs[c % len(engs)]
                kc = "%s.%d.%d" % (key, stg_n[0], c)
                wk = [key, kc]
                if en == "act":
                    if gain_pc is not None:
                        S.op("act", lambda e, c=c: e.activation(out=dst_bf[:, c, :], in_=sv[:, c, :], func=AF.Copy, scale=gain_pc[:, c:c + 1]),
                             r=[sk], w=[kc], c=ncols / 512 * 0.6)
                    else:
                        S.op("act", lambda e, c=c: e.activation(out=dst_bf[:, c, :], in_=sv[:, c, :], func=AF.Copy), r=[sk], w=[kc], c=ncols / 512 * 0.6)
                else:
                    if gain_pc is not None:
                        S.op(en, lambda e, c=c: e.tensor_scalar_mul(out=dst_bf[:, c, :], in0=sv[:, c, :], scalar1=gain_pc[:, c:c + 1]),
                             r=[sk], w=[kc], c=ncols / 512 * 0.5)
                    else:
                        S.op(en, lambda e, c=c: e.tensor_copy(out=dst_bf[:, c, :], in_=sv[:, c, :]), r=[sk], w=[kc], c=ncols / 512 * 0.5)
            pw_keys.setdefault(key, []).extend("%s.%d.%d" % (key, stg_n[0], c) for c in range(nchunk))

        def prep_p3():
            Wout = A.alloc_top([8, 1024], BF16)
            Wr = A.alloc_top([8, 20])
            gffnb = A.alloc_top([1024])
            for nb in range(2):
                prep_weight(Wout[:, :, nb * 512:(nb + 1) * 512],
                            w_out[:, nb * 512:(nb + 1) * 512].rearrange("(c p) f -> p c f", p=128), 8, 512, gao, "Wout")
            S.op("sp", lambda e: e.dma_start(out=RS(stg[:, 0:160], 8, 20), in_=w_r.rearrange("(c p) f -> p c f", p=128)),
                 w=["stg0"], dma=("ld", "stg0"))
            for c in range(8):
                S.op("dve", lambda e, c=c: e.tensor_scalar_mul(out=Wr[:, c, :], in0=RS(stg[:, 0:160], 8, 20)[:, c, :], scalar1=gffnpc_t[:, c:c + 1]),
                     r=["stg0", "gffnpc"], w=["Wr"], c=0.1)
            ld(gffnb, gffnbd[:, :], "gffnb")
            return Wout, Wr, gffnb

        def prep_p6():
            Wg = A.alloc_top([8, 1024], BF16)
            Wp = A.alloc_top([2, 1024], BF16)
            gpostb = A.alloc_top([1024])
            for nb in range(2):
                prep_weight(Wg[:, :, nb * 512:(nb + 1) * 512],
                            w_pg[:, nb * 512:(nb + 1) * 512].rearrange("(c p) f -> p c f", p=128), 8, 512, gplepc_t, "Wg")
            prep_weight(Wp, w_pp.rearrange("(c p) f -> p c f", p=128), 2, 1024, None, "Wp")
            ld(gpostb, gpostbd[:, :], "gpostb")
            return Wg, Wp, gpostb

        if upto >= 1:
            Win = A.alloc([8, 3072], BF16)
            for nb in range(6):
                prep_weight(Win[:, :, nb * 512:(nb + 1) * 512],
                            w_in[:, nb * 512:(nb + 1) * 512].rearrange("(c p) f -> p c f", p=128),
                            8, 512, gmix, "Win")
            gqb = const_f(gqd[:, :], [512], "gqb")
            gkb = const_f(gkd[:, :], [512], "gkb")
            xt = [A.alloc([1024]) for _ in range(2)]
            junk = A.alloc([1024])
            junk2 = [A.alloc([512]) for _ in range(2)]
            ss = A.alloc([2]); rstd = A.alloc([2])
            xb = [A.alloc([1024], BF16) for _ in range(2)]
            xnT = [A.alloc([8, 512], BF16) for _ in range(2)]
            qn = [A.alloc([512]) for _ in range(2)]
            ss8 = [A.alloc([8]) for _ in range(2)]
            r8 = [A.alloc([8, 1]) for _ in range(2)]
            rt = [[A.alloc([8, 8]) for _ in range(4)] for _ in range(2)]
            qb = [A.alloc([512], BF16) for _ in range(2)]
            qTs = A.alloc([4, 512], BF16)
            kTs = A.alloc([4, 512], BF16)
            vst = [A.alloc([4, 3, 64], BF16) for _ in range(2)]
            chs = A.alloc([512])
            ut = A.alloc([4, 514])
            yt = A.alloc([512])
            ot = A.alloc([512])
            sqb = A.alloc([512], BF16)
            rsq = A.alloc([512])
            cst = A.alloc([4, 512], BF16)
            S.op("pool", lambda e: e.memset(ut.rearrange("p a b -> p (a b)"), 0.0), w=["ut"])
            B_TR, B_Q, B_K, B_V, B_TQ, B_CB, B_CC, B_CH = range(8)
            ptr = RS(PB(B_TR), 8, 128)
            ptq = RS(PB(B_TQ)[:, 0:512], 4, 128)

            def qk_proc(pz, gain_b, j, stage, slot, w):
                jk = junk2[w]; q_n = qn[w]; q_b = qb[w]
                S.op("act", lambda e: e.activation(out=jk, in_=pz, func=AF.Square), r=["pz%d" % w], w=["jk%d" % w])
                S.op("dve", lambda e: e.reduce_sum(out=ss8[w], in_=RS(jk, 8, 64), axis=AX.X), r=["jk%d" % w], w=["ss8%d" % w])
                r8f = r8[w].rearrange("p a b -> p (a b)")
                S.op("act", lambda e: e.activation(out=r8f, in_=ss8[w], func=AF.Ln, scale=1.0 / 64, bias=epst[:, 0:1]),
                     r=["ss8%d" % w, "epst"], w=["r8%d" % w], c=0.2)
                S.op("act", lambda e: e.activation(out=r8f, in_=r8f, func=AF.Exp, scale=-0.5), r=["r8%d" % w], w=["r8%d" % w], c=0.2)
                S.op("dve", lambda e: e.tensor_tensor(out=RS(q_n, 8, 64), in0=RS(pz, 8, 64), in1=r8[w].to_broadcast([128, 8, 64]), op=ALU.mult),
                     r=["pz%d" % w, "r8%d" % w], w=["qn%d" % w])
                S.op("dve", lambda e: e.tensor_tensor(out=q_n, in0=q_n, in1=gain_b, op=ALU.mult),
                     r=["qn%d" % w, "gqb", "gkb"], w=["qn%d" % w], c=0.45)
                q3 = RS(q_n, 8, 64); b3 = RS(q_b, 8, 64)
                cb = cost[:, j:j + 1, :].to_broadcast([128, 8, 8]); sb = sint[:, j:j + 1, :].to_broadcast([128, 8, 8])
                t1, t2, t3, t4 = rt[w]
                kq = ["qn%d" % w, "cos", "sin"]
                S.op("dve", lambda e: e.tensor_tensor(out=t1, in0=q3[:, :, 0:8], in1=cb, op=ALU.mult), r=kq, w=["t1%d" % w])
                S.op("pool", lambda e: e.tensor_tensor(out=t2, in0=q3[:, :, 8:16], in1=sb, op=ALU.mult), r=kq, w=["t2%d" % w])
                S.op("dve", lambda e: e.tensor_tensor(out=t3, in0=q3[:, :, 8:16], in1=cb, op=ALU.mult), r=kq, w=["t3%d" % w])
                S.op("pool", lambda e: e.tensor_tensor(out=t4, in0=q3[:, :, 0:8], in1=sb, op=ALU.mult), r=kq, w=["t4%d" % w])
                S.op("dve", lambda e: e.tensor_tensor(out=b3[:, :, 0:8], in0=t1, in1=t2, op=ALU.subtract),
                     r=["t1%d" % w, "t2%d" % w], w=["qb%d" % w])
                S.op("dve", lambda e: e.tensor_tensor(out=b3[:, :, 8:16], in0=t3, in1=t4, op=ALU.add),
                     r=["t3%d" % w, "t4%d" % w], w=["qb%d" % w])
                S.op("act", lambda e: e.activation(out=b3[:, :, 16:64], in_=q3[:, :, 16:64], func=AF.Copy), r=["qn%d" % w], w=["qb%d" % w], c=0.5)
                for pr in range(4):
                    S.op("pe", lambda e, pr=pr: e.transpose(out=ptq[:, pr, :], in_=q_b[:, pr * 128:(pr + 1) * 128], identity=identb),
                         r=["qb%d" % w, "identb"], w=["ptq"], c=0.11)
                S.op("act", lambda e: e.activation(out=stage[:, :, slot * 128:(slot + 1) * 128], in_=ptq, func=AF.Copy),
                     r=["ptq"], w=["stage%d" % w])

            for j in P1_RANGE:
                g, s = divmod(j, 4)
                own = j >= 16
                xi = j % 2
                S.op("sp", lambda e, j=j, xi=xi: e.dma_start(out=xt[xi], in_=xin[j * 128:(j + 1) * 128, :]),
                     w=["xt%d" % xi], dma=("xt", xi))
                S.op("act", lambda e, xi=xi: e.activation(out=junk, in_=xt[xi], func=AF.Square, accum_out=ss[:, 0:1]),
                     r=["xt%d" % xi], w=["junk", "ss"], c=1.05)
                S.op("act", lambda e: e.activation(out=rstd[:, 0:1], in_=ss[:, 0:1], func=AF.Ln, scale=1.0 / 1024, bias=epst[:, 0:1]),
                     r=["ss", "epst"], w=["rstd"], c=0.2)
                S.op("act", lambda e: e.activation(out=rstd[:, 0:1], in_=rstd[:, 0:1], func=AF.Exp, scale=-0.5), r=["rstd"], w=["rstd"], c=0.2)
                S.op("dve", lambda e, xi=xi: e.tensor_scalar_mul(out=xb[xi], in0=xt[xi], scalar1=rstd[:, 0:1]),
                     r=["xt%d" % xi, "rstd"], w=["xb%d" % xi], c=0.7)
                for c in range(8):
                    S.op("pe", lambda e, c=c, xi=xi: e.transpose(out=ptr[:, c, :], in_=xb[xi][:, c * 128:(c + 1) * 128], identity=identb),
                         r=["xb%d" % xi, "identb"], w=["ptr"], c=0.11)
                xg = xnT[g % 2]
                S.op("act", lambda e, xg=xg, s=s: e.activation(out=xg[:, :, s * 128:(s + 1) * 128], in_=ptr, func=AF.Copy),
                     r=["ptr"], w=["xnT%d" % (g % 2)])
                nbs = ([0] if own else []) + [1, 2]
                for c in range(8):
                    for nb in nbs:
                        S.op("pe", lambda e, c=c, nb=nb, xg=xg, s=s: e.matmul(
                            banks[B_Q + nb][:], lhsT=xg[:, c, s * 128:(s + 1) * 128], rhs=Win[:, c, nb * 512:(nb + 1) * 512],
                            start=(c == 0), stop=(c == 7)),
                            r=["xnT%d" % (g % 2)] + pw_keys["Win"], w=["pz%d" % nb if nb < 2 else "pv"])
                if own:
                    qk_proc(banks[B_Q][:], gqb, j, qTs, s, 0)
                qk_proc(banks[B_K][:], gkb, j, kTs, s, 1)
                vi = j % 2
                v4 = RS(banks[B_V][:], 4, 2, 64)
                S.op("act", lambda e, vi=vi, v4=v4: e.activation(out=vst[vi][:, :, 0:3:2, :], in_=v4, func=AF.Copy),
                     r=["pv"], w=["vst%d" % vi])
                S.op("pool", lambda e, vi=vi, j=j: e.tensor_copy(out=vst[vi][:, :, 1, :], in_=RS(ext, 48, 1)[:, j:j + 1, :].to_broadcast([128, 4, 64])),
                     r=["ex"], w=["vst%d" % vi])
                S.op("sp", lambda e, vi=vi, j=j: e.dma_start(out=Vs2[:, j * 128:(j + 1) * 128, :].rearrange("a p c -> p a c"),
                                                             in_=vst[vi].rearrange("p a b c -> p a (b c)")),
                     r=["vst%d" % vi], dma=("vst", vi))
                S.op("sp", lambda e, vi=vi, j=j: e.dma_start(out=Vs1[:, :, j * 192:(j + 1) * 192].rearrange("a p c -> p a c"),
                                                             in_=vst[vi].rearrange("p a b c -> p a (b c)")),
                     r=["vst%d" % vi], dma=("vst", vi))
                if s == 3:
                    if own:
                        S.op("sp", lambda e, g=g: e.dma_start(out=QT[:, :, (g - 4) * 512:(g - 3) * 512].rearrange("a p t -> p a t"), in_=qTs),
                             r=["stage0"], dma=("qTs",))
                    S.op("sp", lambda e, g=g: e.dma_start(out=KT[:, :, g * 512:(g + 1) * 512].rearrange("a p t -> p a t"), in_=kTs),
                         r=["stage1"], dma=("kTs",))
                    if g >= 3:
                        for ci in range(4):
                            for bi, (bk, coff) in enumerate(((B_CB, 1536), (B_CC, 2048), (B_CH, 2560))):
                                col = coff + ci * 128
                                for c in range(8):
                                    S.op("pe", lambda e, c=c, bk=bk, col=col, xg=xg: e.matmul(
                                        banks[bk][:], lhsT=Win[:, c, col:col + 128], rhs=xg[:, c, :], start=(c == 0), stop=(c == 7)),
                                        r=["xnT%d" % (g % 2)] + pw_keys["Win"], w=["pc%d" % bi])
                            S.op("act", lambda e: e.activation(out=chs, in_=banks[B_CH][:], func=AF.Copy), r=["pc2"], w=["chs"])
                            S.op("dve", lambda e, ci=ci: e.tensor_tensor(out=ut[:, ci, 2:514], in0=banks[B_CC][:], in1=chs, op=ALU.mult),
                                 r=["pc1", "chs"], w=["ut"])
                            if g >= 4:
                                S.op("dve", lambda e, ci=ci: e.tensor_scalar_mul(out=yt, in0=ut[:, ci, 2:514], scalar1=cw[:, ci, 2:3]),
                                     r=["ut", "cw"], w=["yt"])
                                S.op("dve", lambda e, ci=ci: e.scalar_tensor_tensor(out=yt, in0=ut[:, ci, 1:513], scalar=cw[:, ci, 1:2], in1=yt, op0=ALU.mult, op1=ALU.add),
                                     r=["ut", "cw", "yt"], w=["yt"])
                                S.op("dve", lambda e, ci=ci: e.scalar_tensor_tensor(out=yt, in0=ut[:, ci, 0:512], scalar=cw[:, ci, 0:1], in1=yt, op0=ALU.mult, op1=ALU.add),
                                     r=["ut", "cw", "yt"], w=["yt"])
                                S.op("dve", lambda e: e.tensor_tensor(out=ot, in0=yt, in1=banks[B_CB][:], op=ALU.mult),
                                     r=["yt", "pc0"], w=["ot"])
                                S.op("act", lambda e: e.activation(out=sqb, in_=ot, func=AF.Square), r=["ot"], w=["sqb"])
                                S.op("pe", lambda e: e.matmul(banks[B_CC][:], lhsT=blk64, rhs=sqb, start=True, stop=True),
                                     r=["sqb", "blk64", "ut"], w=["pc1"])
                                S.op("act", lambda e: e.activation(out=rsq, in_=banks[B_CC][:], func=AF.Ln, bias=epst[:, 0:1]),
                                     r=["pc1", "epst"], w=["rsq"], c=0.6)
                                S.op("act", lambda e: e.activation(out=rsq, in_=rsq, func=AF.Exp, scale=-0.5), r=["rsq"], w=["rsq"], c=0.6)
                                S.op("pool", lambda e, ci=ci: e.tensor_tensor(out=cst[:, ci, :], in0=ot, in1=rsq, op=ALU.mult),
                                     r=["ot", "rsq"], w=["cst"])
                            S.op("pool", lambda e, ci=ci: e.tensor_copy(out=ut[:, ci, 0:2], in_=ut[:, ci, 512:514]), r=["ut"], w=["ut"])
                        if g >= 4:
                            S.op("sp", lambda e, g=g: e.dma_start(out=CT[:, :, (g - 4) * 512:(g - 3) * 512].rearrange("a p t -> p a t"), in_=cst),
                                 r=["cst"], dma=("cst",))
            S.barrier()
        A.reset(base_mark)

        if upto >= 2:
            p3w = prep_p3() if upto >= 3 else None
            qTz = A.alloc([2, NTOK], BF16)
            kT = A.alloc([NALL], BF16)
            qTz16 = A.alloc([2, 16, NTOK // 16], BF16)
            kT16 = A.alloc([16, NALL // 16], BF16)
            S.op("pool", lambda e: e.memset(qTz[64:128, 0, :], 0.0), w=["qT"])
            S.op("pool", lambda e: e.memset(qTz[0:64, 1, :], 0.0), w=["qT"])
            Vd = [A.alloc([48 * 192], BF16) for _ in range(2)]
            acc = A.alloc([2, NTOK])
            Pt = [A.alloc([4, 128], BF16) for _ in range(4)]
            sq2 = [A.alloc([2, 512], BF16) for _ in range(2)]
            rs2 = A.alloc([512])
            ats = [A.alloc([512], BF16) for _ in range(2)]
            it = 0
            vcount = 0
            for pr in range(4):
                S.op("sp", lambda e, pr=pr: e.dma_start(out=qTz[0:64, 0, :], in_=QT[pr, 0:64, :]), w=["qT"], dma=("qT",))
                S.op("sp", lambda e, pr=pr: e.dma_start(out=qTz[64:128, 1, :], in_=QT[pr, 64:128, :]), w=["qT"], dma=("qT",))
                S.op("sp", lambda e, pr=pr: e.dma_start(out=kT, in_=KT[pr, :, :]), w=["kT"], dma=("kT",))
                perm_chunks = []
                if 16 in BRANCHES:
                    for hh in range(2):
                        for g4 in range(4):
                            rs_ = slice(g4 * 4, (g4 + 1) * 4)
                            perm_chunks.append(lambda hh=hh, g4=g4, rs_=rs_: S.op(
                                "dve", lambda e: e.tensor_copy(out=qTz16[:, hh, rs_, :],
                                                               in_=qTz[:, hh, :].rearrange("p (n r) -> p r n", r=16)[:, rs_, :]),
                                r=["qT"], w=["qT16.%d.%d" % (hh, g4)], c=1.2))
                    for g2 in range(8):
                        rs_ = slice(g2 * 2, (g2 + 1) * 2)
                        perm_chunks.append(lambda g2=g2, rs_=rs_: S.op(
                            "act", lambda e: e.activation(out=kT16[:, rs_, :], in_=kT.rearrange("p (n r) -> p r n", r=16)[:, rs_, :], func=AF.Copy),
                            r=["kT"], w=["kT16.%d" % g2], c=1.5))
                for bi, d in enumerate(BRANCHES):
                    vb = vcount % 2
                    vcount += 1
                    nblk = 48 // d
                    Vv = RS(Vd[vb], nblk, d, 192)
                    if d == 1:
                        S.op("sp", lambda e, vb=vb, pr=pr: e.dma_start(out=Vd[vb], in_=Vs1[pr, :, :]),
                             w=["Vd%d" % vb], dma=("Vd", vb), c=6.0)
                    else:
                        src = Vs2[pr, :, :].rearrange("(blk i r) c -> i blk (r c)", i=128, r=d)
                        dst = RS(Vd[vb], nblk, d * 192)
                        for b0 in range(0, nblk, 6):
                            b1 = min(nblk, b0 + 6)
                            S.op("sp", lambda e, dst=dst, src=src, b0=b0, b1=b1: e.dma_start(out=dst[:, b0:b1, :], in_=src[:, b0:b1, :]),
                                 w=["Vd%d" % vb], dma=("Vd", vb), c=6.0)
                    for qblk in range(16 // d, 48 // d):
                        for r in range(d):
                            if P2_STOP < 2:
                                continue
                            if d == 16:
                                while perm_chunks:
                                    perm_chunks.pop(0)()
                            elif it % 4 == 3 and perm_chunks:
                                perm_chunks.pop(0)()
                            pi = it % 4
                            it += 1
                            ps = RS(banks[pi][:], 4, 128)
                            qi = (it - 1) % 3
                            po = RS(banks[4 + qi][:, 0:256], 2, 128)
                            q0 = qblk * 128 * d + r - NHALO
                            qsl = slice(q0, q0 + 127 * d + 1, d)
                            kp0 = (qblk - 1) * 128 * d + r
                            kc0 = qblk * 128 * d + r
                            ksl = [slice(kp0, kp0 + 127 * d + 1, d), slice(kc0, kc0 + 127 * d + 1, d)]
                            S.op("pe", lambda e, pi=pi: e.matmul(banks[pi][:], lhsT=identb, rhs=mask4, start=True, stop=False),
                                 r=["mask4", "identb"], w=["ps%d" % pi], c=0.25)
                            for hh in range(2):
                                prt = slice(hh * 64, hh * 64 + 64)
                                for kk in range(2):
                                    if d == 16:
                                        n0 = qblk * 128 - NHALO // 16
                                        kb0 = (qblk - 1 + kk) * 128
                                        S.op("pe", lambda e, ps=ps, hh=hh, kk=kk, r=r, n0=n0, kb0=kb0: e.matmul(
                                            ps[:, hh * 2 + kk, :], lhsT=kT16[:, r, kb0:kb0 + 128], rhs=qTz16[:, hh, r, n0:n0 + 128],
                                            start=False, stop=(hh == 1 and kk == 1)),
                                            r=["kT16.%d" % (r // 2), "qT16.%d.%d" % (hh, r // 4)], w=["ps%d" % pi], c=0.06)
                                    else:
                                        S.op("pe", lambda e, ps=ps, hh=hh, kk=kk, ksl=ksl, qsl=qsl: e.matmul(
                                            ps[:, hh * 2 + kk, :], lhsT=kT[:, ksl[kk]], rhs=qTz[:, hh, qsl], start=False, stop=(hh == 1 and kk == 1)),
                                            r=["kT", "qT"], w=["ps%d" % pi], c={1: 0.06, 4: 0.115}[d])
                            P = Pt[pi]
                            S.op("act", lambda e, P=P, ps=ps: e.activation(out=P, in_=ps, func=AF.Exp, scale=0.125),
                                 r=["ps%d" % pi], w=["P%d" % pi], c=0.62)
                            if P2_STOP < 3:
                                continue
                            for hh in range(2):
                                vsl = slice(0, 128) if hh == 0 else slice(64, 192)
                                for kk in range(2):
                                    S.op("pe", lambda e, po=po, hh=hh, kk=kk, vsl=vsl, Vv=Vv, qblk=qblk, r=r, P=P: e.matmul(
                                        po[:, hh, :], lhsT=Vv[:, qblk - 1 + kk, r, vsl], rhs=P[:, hh * 2 + kk, :],
                                        start=(kk == 0), stop=(kk == 1)),
                                        r=["P%d" % pi, "Vd%d" % vb], w=["po%d" % qi], c=0.06)
                            ak = ["acc%d" % gg for gg in range(q0 // 512, (q0 + 127 * d) // 512 + 1)]
                            if bi == 0:
                                S.op("dve", lambda e, po=po, qsl=qsl: e.tensor_copy(out=acc[:, :, qsl], in_=po),
                                     r=["po%d" % qi], w=ak, c=0.45)
                            else:
                                S.op("dve", lambda e, po=po, qsl=qsl: e.tensor_tensor(out=acc[:, :, qsl], in0=acc[:, :, qsl], in1=po, op=ALU.add),
                                     r=["po%d" % qi] + ak, w=ak, c=0.62)
                for tg in range(8 if P2_STOP >= 4 else 0):
                    fi = tg % 2
                    csl = slice(tg * 512, (tg + 1) * 512)
                    S.op("act", lambda e, fi=fi, csl=csl: e.activation(out=sq2[fi], in_=acc[:, :, csl], func=AF.Square),
                         r=["acc%d" % tg], w=["sq2%d" % fi])
                    S.op("pe", lambda e, fi=fi: e.matmul(banks[7][:], lhsT=wreda, rhs=sq2[fi][:, 0, :], start=True, stop=False),
                         r=["sq2%d" % fi, "wreda"], w=["pn"])
                    S.op("pe", lambda e, fi=fi: e.matmul(banks[7][:], lhsT=wredb, rhs=sq2[fi][:, 1, :], start=False, stop=True),
                         r=["sq2%d" % fi, "wredb"], w=["pn"])
                    S.op("act", lambda e: e.activation(out=rs2, in_=banks[7][:], func=AF.Ln), r=["pn"], w=["rs2"], c=0.6)
                    S.op("act", lambda e: e.activation(out=rs2, in_=rs2, func=AF.Exp, scale=-0.5), r=["rs2"], w=["rs2"], c=0.6)
                    S.op("dve", lambda e, fi=fi, csl=csl: e.tensor_tensor(out=ats[fi][0:64, :], in0=acc[0:64, 0, csl], in1=rs2[0:64, :], op=ALU.mult),
                         r=["acc%d" % tg, "rs2"], w=["ats%d" % fi])
                    S.op("dve", lambda e, fi=fi, csl=csl: e.tensor_tensor(out=ats[fi][64:128, :], in0=acc[64:128, 1, csl], in1=rs2[64:128, :], op=ALU.mult),
                         r=["acc%d" % tg, "rs2"], w=["ats%d" % fi])
                    S.op("sp", lambda e, fi=fi, csl=csl, pr=pr: e.dma_start(out=AT[pr, :, csl], in_=ats[fi]),
                         r=["ats%d" % fi], dma=("ats", fi))
            S.barrier()
        A.reset(base_mark)

        if upto >= 3:
            Wout, Wr, gffnb = p3w
            atl = [A.alloc([4, 512], BF16) for _ in range(2)]
            ctl = [A.alloc([4, 512], BF16) for _ in range(2)]
            xt3 = [A.alloc([1024]) for _ in range(2)]
            ht = [A.alloc([1024]) for _ in range(2)]
            xf = [A.alloc([1024]) for _ in range(2)]
            xfT = A.alloc([8, 128])
            xfb_all = A.alloc_top([32, 1024], BF16)
            xfb_keep = A.n
            junk3 = A.alloc([1024])
            ss3 = A.alloc([2]); rstd3 = A.alloc([2])
            for jt in range(32):
                g, s = divmod(jt, 4)
                gi = g % 2
                bi2 = jt % 2
                if s == 0:
                    S.op("sp", lambda e, g=g, gi=gi: e.dma_start(out=atl[gi], in_=AT[:, :, g * 512:(g + 1) * 512].rearrange("a p t -> p a t")),
                         w=["atl%d" % gi], dma=("atl", gi))
                    S.op("sp", lambda e, g=g, gi=gi: e.dma_start(out=ctl[gi], in_=CT[:, :, g * 512:(g + 1) * 512].rearrange("a p t -> p a t")),
                         w=["ctl%d" % gi], dma=("ctl", gi))
                S.op("sp", lambda e, jt=jt, bi2=bi2: e.dma_start(out=xt3[bi2], in_=xin[NHALO + jt * 128:NHALO + (jt + 1) * 128, :]),
                     w=["xt3%d" % bi2], dma=("xt3", bi2))
                ph = banks[2 * bi2:2 * bi2 + 2]
                for half in range(2):
                    for c in range(8):
                        src = atl[gi] if c < 4 else ctl[gi]
                        S.op("pe", lambda e, half=half, c=c, src=src, s=s, ph=ph: e.matmul(
                            ph[half][:], lhsT=src[:, c % 4, s * 128:(s + 1) * 128], rhs=Wout[:, c, half * 512:(half + 1) * 512],
                            start=(c == 0), stop=(c == 7)),
                            r=["atl%d" % gi, "ctl%d" % gi] + pw_keys["Wout"], w=["ph%d%d" % (bi2, half)])
                for half in range(2):
                    hs = slice(half * 512, (half + 1) * 512)
                    S.op("dve", lambda e, half=half, hs=hs, bi2=bi2, ph=ph: e.tensor_tensor(out=ht[bi2][:, hs], in0=xt3[bi2][:, hs], in1=ph[half][:], op=ALU.add),
                         r=["xt3%d" % bi2, "ph%d%d" % (bi2, half)], w=["ht%d" % bi2])
                S.op("sp", lambda e, jt=jt, bi2=bi2: e.dma_start(out=Hd[jt * 128:(jt + 1) * 128, :], in_=ht[bi2]),
                     r=["ht%d" % bi2], dma=("ht", bi2))
                S.op("act", lambda e, bi2=bi2: e.activation(out=junk3, in_=ht[bi2], func=AF.Square, accum_out=ss3[:, 0:1]),
                     r=["ht%d" % bi2], w=["junk3", "ss3"], c=1.05)
                S.op("act", lambda e: e.activation(out=rstd3[:, 0:1], in_=ss3[:, 0:1], func=AF.Sqrt, scale=1.0 / 1024, bias=epst[:, 0:1]),
                     r=["ss3", "epst"], w=["rstd3"])
                S.op("dve", lambda e: e.reciprocal(out=rstd3[:, 0:1], in_=rstd3[:, 0:1]), r=["rstd3"], w=["rstd3"])
                S.op("dve", lambda e, bi2=bi2: e.tensor_scalar_mul(out=xf[bi2], in0=ht[bi2], scalar1=rstd3[:, 0:1]),
                     r=["ht%d" % bi2, "rstd3"], w=["xf%d" % bi2])
                S.op("dve", lambda e, bi2=bi2, jt=jt: e.tensor_tensor(out=xfb_all[:, jt, :], in0=xf[bi2], in1=gffnb, op=ALU.mult),
                     r=["xf%d" % bi2, "gffnb"], w=["xfball%d" % jt], c=0.7)
                pxT = [RS(banks[4][:], 4, 128), RS(banks[5][:], 4, 128)]
                for c in range(8):
                    S.op("pe", lambda e, c=c, bi2=bi2, pxT=pxT: e.matmul(pxT[c // 4][:, c % 4, :], lhsT=xf[bi2][:, c * 128:(c + 1) * 128], rhs=identf, start=True, stop=True),
                         r=["xf%d" % bi2, "identf"], w=["pxT"])
                for hh in range(2):
                    S.op("act", lambda e, hh=hh, pxT=pxT: e.activation(out=xfT[:, hh * 4:(hh + 1) * 4, :], in_=pxT[hh], func=AF.Copy),
                         r=["pxT"], w=["xfT"])
                for c in range(8):
                    S.op("pe", lambda e, c=c: e.matmul(banks[6][:, 0:20], lhsT=xfT[:, c, :], rhs=Wr[:, c, :], start=(c == 0), stop=(c == 7)),
                         r=["xfT", "Wr"], w=["plg"])
                S.op("dve", lambda e, jt=jt: e.tensor_tensor(out=lgall[:, jt, :], in0=banks[6][:, 0:20], in1=rbias, op=ALU.add),
                     r=["plg", "rbias"], w=["lgall"])
            S.barrier()
        A.reset(base_mark)
        A.free_top()
        if upto >= 3:
            A.n = xfb_keep

        if upto >= 4:
            T2 = lambda *d: A.alloc(list(d))
            lg = lgall[:, :, 0:4]
            le4 = lgall[:, :, 4:20]
            mg = T2(32, 1); ohg = T2(32, 4); dg = T2(32, 4); sg = T2(32); pgt = T2(32)
            lsel = T2(32, 4); tmp4 = T2(32, 4)
            m1 = T2(32, 1); oh1 = T2(32, 4); l2 = T2(32, 4); m2 = T2(32, 1); oh2 = T2(32, 4)
            dm = T2(32); ed = T2(32); aa = T2(32)
            oh16 = [T2(32, 16), T2(32, 16)]
            Mf = T2(32, 16); Mb = A.alloc([512], BF16)
            Rk = T2(32, 16); Cn = T2(32, 16); Pj = T2(32, 16)
            ntot = T2(16, 1); cmp17 = T2(16, 17); npad = T2(16); offe = T2(16); endp = T2(16)
            cmpT = T2(NT, 16); eid = T2(NT); widf = T2(NT)
            basep = T2(32, 16); tmp16 = T2(32, 16); posf = T2(2, 32)
            K = "rt"
            dv = lambda fn: S.op("dve", fn, r=[K, "lgall", "thr", "tstart", "pidx"], w=[K])
            fl = lambda t: t.rearrange("p a b -> p (a b)")
            dv(lambda e: e.reduce_max(out=fl(mg), in_=lg, axis=AX.X))
            dv(lambda e: e.tensor_tensor(out=ohg, in0=lg, in1=mg.to_broadcast([128, 32, 4]), op=ALU.is_equal))
            dv(lambda e: e.tensor_tensor(out=dg, in0=lg, in1=mg.to_broadcast([128, 32, 4]), op=ALU.subtract))
            S.op("act", lambda e: e.activation(out=dg, in_=dg, func=AF.Exp), r=[K], w=[K])
            dv(lambda e: e.reduce_sum(out=sg, in_=dg, axis=AX.X))
            dv(lambda e: e.reciprocal(out=pgt, in_=sg))
            for gq_ in range(4):
                dst = lsel if gq_ == 0 else tmp4
                dv(lambda e, gq_=gq_, dst=dst: e.tensor_tensor(out=dst, in0=le4[:, :, gq_ * 4:gq_ * 4 + 4], in1=ohg[:, :, gq_:gq_ + 1].to_broadcast([128, 32, 4]), op=ALU.mult))
                if gq_ > 0:
                    dv(lambda e: e.tensor_tensor(out=lsel, in0=lsel, in1=tmp4, op=ALU.add))
            dv(lambda e: e.reduce_max(out=fl(m1), in_=lsel, axis=AX.X))
            dv(lambda e: e.tensor_tensor(out=oh1, in0=lsel, in1=m1.to_broadcast([128, 32, 4]), op=ALU.is_equal))
            dv(lambda e: e.scalar_tensor_tensor(out=l2, in0=oh1, scalar=-1e30, in1=lsel, op0=ALU.mult, op1=ALU.add))
            dv(lambda e: e.reduce_max(out=fl(m2), in_=l2, axis=AX.X))
            dv(lambda e: e.tensor_tensor(out=oh2, in0=l2, in1=m2.to_broadcast([128, 32, 4]), op=ALU.is_equal))
            dv(lambda e: e.tensor_tensor(out=dm, in0=fl(m2), in1=fl(m1), op=ALU.subtract))
            S.op("act", lambda e: e.activation(out=ed, in_=dm, func=AF.Exp), r=[K], w=[K])
            dv(lambda e: e.tensor_scalar_add(out=aa, in0=ed, scalar1=1.0))
            dv(lambda e: e.reciprocal(out=aa, in_=aa))
            dv(lambda e: e.tensor_tensor(out=gates[:, 0, :], in0=pgt, in1=aa, op=ALU.mult))
            dv(lambda e: e.tensor_tensor(out=ed, in0=ed, in1=aa, op=ALU.mult))
            dv(lambda e: e.tensor_tensor(out=gates[:, 1, :], in0=pgt, in1=ed, op=ALU.mult))
            for sl, ohs in enumerate((oh1, oh2)):
                for gq_ in range(4):
                    dv(lambda e, sl=sl, ohs=ohs, gq_=gq_: e.tensor_tensor(out=oh16[sl][:, :, gq_ * 4:gq_ * 4 + 4], in0=ohs, in1=ohg[:, :, gq_:gq_ + 1].to_broadcast([128, 32, 4]), op=ALU.mult))
            dv(lambda e: e.tensor_tensor(out=Mf, in0=oh16[0], in1=oh16[1], op=ALU.add))
            dv(lambda e: e.tensor_copy(out=Mb, in_=fl(Mf)))
            S.op("pe", lambda e: e.matmul(banks[0][:], lhsT=triu, rhs=Mb, start=True, stop=True), r=[K, "triu"], w=["prk"])
            S.op("pe", lambda e: e.matmul(banks[1][:], lhsT=onesb, rhs=Mb, start=True, stop=True), r=[K, "onesb"], w=["pcn"])
            S.op("dve", lambda e: e.tensor_copy(out=fl(Rk), in_=banks[0][:]), r=["prk", K], w=[K])
            S.op("dve", lambda e: e.tensor_copy(out=fl(Cn), in_=banks[1][:]), r=["pcn", K], w=[K])
            S.op("pool", lambda e: e.memset(fl(Pj), 0.0), r=[K], w=[K])
            S.op("pool", lambda e: e.memset(offe, 0.0), r=[K], w=[K])
            for j in range(1, 32):
                dv(lambda e, j=j: e.tensor_tensor(out=Pj[:, j, :], in0=Pj[:, j - 1, :], in1=Cn[:, j - 1, :], op=ALU.add))
            nt1 = fl(ntot)
            dv(lambda e: e.tensor_tensor(out=nt1, in0=Pj[:, 31, :], in1=Cn[:, 31, :], op=ALU.add))
            dv(lambda e: e.tensor_scalar_add(out=nt1, in0=nt1, scalar1=float(TS - 1)))
            dv(lambda e: e.tensor_tensor(out=cmp17, in0=ntot.to_broadcast([128, 16, 17]), in1=thr[:, None, :].to_broadcast([128, 16, 17]) if False else RS(thr, 1, 17).to_broadcast([128, 16, 17]), op=ALU.is_ge))
            dv(lambda e: e.reduce_sum(out=npad, in_=cmp17, axis=AX.X))
            dv(lambda e: e.tensor_scalar_mul(out=npad, in0=npad, scalar1=float(TS)))
            for ei in range(1, 16):
                dv(lambda e, ei=ei: e.tensor_tensor(out=offe[:, ei:ei + 1], in0=offe[:, ei - 1:ei], in1=npad[:, ei - 1:ei], op=ALU.add))
            dv(lambda e: e.tensor_tensor(out=endp, in0=offe, in1=npad, op=ALU.add))
            dv(lambda e: e.tensor_tensor(out=cmpT, in0=RS(tstart, NT, 1).to_broadcast([128, NT, 16]), in1=RS(endp, 1, 16).to_broadcast([128, NT, 16]), op=ALU.is_ge))
            dv(lambda e: e.reduce_sum(out=eid, in_=cmpT, axis=AX.X))
            dv(lambda e: e.tensor_scalar_min(out=eid, in0=eid, scalar1=15.0))
            dv(lambda e: e.tensor_scalar(out=widf, in0=eid, scalar1=128.0, scalar2=pidx[:, 0:1], op0=ALU.mult, op1=ALU.add))
            dv(lambda e: e.tensor_copy(out=widx_i, in_=widf))
            dv(lambda e: e.tensor_tensor(out=basep, in0=Pj, in1=Rk, op=ALU.add))
            dv(lambda e: e.tensor_tensor(out=basep, in0=basep, in1=RS(offe, 1, 16).to_broadcast([128, 32, 16]), op=ALU.add))
            for sl in range(2):
                dv(lambda e, sl=sl: e.tensor_tensor(out=tmp16, in0=basep, in1=oh16[sl], op=ALU.mult))
                dv(lambda e, sl=sl: e.reduce_sum(out=posf[:, sl, :], in_=tmp16, axis=AX.X))
            dv(lambda e: e.tensor_copy(out=fl(pos_i), in_=fl(posf)))
            if dbg:
                fin.append(S.op("sp", lambda e: e.dma_start(out=dbgo["gates"][:, :], in_=fl(gates)), r=[K], dma=("dbg", 0)))
                fin.append(S.op("sp", lambda e: e.dma_start(out=dbgo["pos"][:, :], in_=fl(pos_i)), r=[K], dma=("dbg", 1)))
                fin.append(S.op("sp", lambda e: e.dma_start(out=dbgo["widx"][:, :], in_=widx_i), r=[K], dma=("dbg", 2)))
                fin.append(S.op("sp", lambda e: e.dma_start(out=dbgo["lg"][:, :], in_=fl(lgall)), r=[K, "lgall"], dma=("dbg", 3)))
            S.barrier()
            zt = A.alloc([4, 1024], BF16)
            S.op("pool", lambda e: e.memset(zt.rearrange("p a b -> p (a b)"), 0.0), w=["zt"])
            for i in range(NT):
                S.op("sp", lambda e, i=i: e.dma_start(out=Xs[i * TS:(i + 1) * TS, :].rearrange("(s p) f -> p s f", p=128), in_=zt),
                     r=["zt"], dma=("zfill",))
            S.barrier()
            for jt in range(32):
                for sl in range(2):
                    S.op("pool", lambda e, jt=jt, sl=sl: e.indirect_dma_start(
                        out=Xs[:, :], out_offset=bass.IndirectOffsetOnAxis(ap=pos_i[:, sl, jt:jt + 1], axis=0),
                        in_=xfb_all[:, jt, :], in_offset=None),
                        r=["xfball%d" % jt], w=["scq%d" % ((2 * jt + sl) % 6)], dma=("xscat", (2 * jt + sl) % 6))
            S.barrier()
            A.free_top()
        A.reset(base_mark)
        rt_mark = A.mark()

        if upto >= 5:
            p6w = prep_p6() if upto >= 6 else None
            W1g = [A.alloc([8, 512], BF16) for _ in range(2)]
            W3g = [A.alloc([8, 512], BF16) for _ in range(2)]
            W2g = [A.alloc([4, 1024], BF16) for _ in range(2)]
            Xt = [A.alloc([4, 1024], BF16) for _ in range(2)]
            XTt = [A.alloc([8, 512], BF16) for _ in range(2)]
            s1 = [A.alloc([512]) for _ in range(2)]
            GT = [A.alloc([4, 512], BF16) for _ in range(2)]
            Yt = [A.alloc([4, 1024]) for _ in range(2)]
            for i in range(NT):
                b = i % 2
                fl3 = lambda t: t.rearrange("p a b -> p (a b)")
                for wi, (wt, wd) in enumerate(((W1g, w1), (W3g, w3), (W2g, w2))):
                    S.op("pool", lambda e, wt=wt, wd=wd, b=b, i=i: e.indirect_dma_start(
                        out=fl3(wt[b]), out_offset=None, in_=wd[:, :],
                        in_offset=bass.IndirectOffsetOnAxis(ap=widx_i[:, i:i + 1], axis=0)),
                        w=["W%d_%d" % (wi, b)], dma=("wg", wi, b))
                S.op("sp", lambda e, b=b, i=i: e.dma_start(out=Xt[b], in_=Xs[i * TS:(i + 1) * TS, :].rearrange("(s p) f -> p s f", p=128)),
                     w=["Xt%d" % b], dma=("Xt", b))
                for s in range(4):
                    pb_ = 6 + (s % 2)
                    ptx = RS(PB(pb_), 8, 128)
                    for c in range(8):
                        S.op("pe", lambda e, s=s, c=c, b=b, ptx=ptx: e.transpose(out=ptx[:, c, :], in_=Xt[b][:, s, c:1024:8], identity=identb),
                             r=["Xt%d" % b, "identb"], w=["ptx%d" % (s % 2)], c=0.11)
                    eng = "act" if s % 2 == 0 else "dve"
                    if eng == "act":
                        S.op("act", lambda e, s=s, b=b, ptx=ptx: e.activation(out=XTt[b][:, :, s * 128:(s + 1) * 128], in_=ptx, func=AF.Copy),
                             r=["ptx%d" % (s % 2)], w=["XT%d" % b])
                    else:
                        S.op("dve", lambda e, s=s, b=b, ptx=ptx: e.tensor_copy(out=XTt[b][:, :, s * 128:(s + 1) * 128], in_=ptx),
                             r=["ptx%d" % (s % 2)], w=["XT%d" % b])
                for c4 in range(4):
                    hb = c4 % 2
                    for wi, wt in enumerate((W1g, W3g)):
                        bk = banks[hb * 2 + wi]
                        for k in range(8):
                            S.op("pe", lambda e, bk=bk, wt=wt, k=k, c4=c4, b=b: e.matmul(
                                bk[:], lhsT=wt[b][:, k, c4:512:4], rhs=XTt[b][:, k, :], start=(k == 0), stop=(k == 7)),
                                r=["XT%d" % b, "W%d_%d" % (wi, b)], w=["phh%d%d" % (hb, wi)])
                    S.op("act", lambda e, hb=hb: e.activation(out=s1[hb], in_=banks[hb * 2][:], func=AF.Silu),
                         r=["phh%d0" % hb], w=["s1%d" % hb])
                    S.op("dve", lambda e, hb=hb, c4=c4, b=b: e.tensor_tensor(out=GT[b][:, c4, :], in0=s1[hb], in1=banks[hb * 2 + 1][:], op=ALU.mult),
                         r=["s1%d" % hb, "phh%d1" % hb], w=["GT%d" % b])
                for s in range(4):
                    for half in range(2):
                        yb = 4 + half
                        for c4 in range(4):
                            S.op("pe", lambda e, yb=yb, s=s, half=half, c4=c4, b=b: e.matmul(
                                banks[yb][:], lhsT=GT[b][:, c4, s * 128:(s + 1) * 128], rhs=W2g[b][:, c4, half * 512:(half + 1) * 512],
                                start=(c4 == 0), stop=(c4 == 3)),
                                r=["GT%d" % b, "W2_%d" % b], w=["py%d" % half])
                        if half == 0:
                            S.op("act", lambda e, yb=yb, s=s, b=b: e.activation(out=Yt[b][:, s, 0:512], in_=banks[yb][:], func=AF.Copy),
                                 r=["py0"], w=["Yt%d" % b])
                        else:
                            S.op("dve", lambda e, yb=yb, s=s, b=b: e.tensor_copy(out=Yt[b][:, s, 512:1024], in_=banks[yb][:]),
                                 r=["py1"], w=["Yt%d" % b])
                S.op("sp", lambda e, b=b, i=i: e.dma_start(out=Ys[i * TS:(i + 1) * TS, :].rearrange("(s p) f -> p s f", p=128), in_=Yt[b]),
                     r=["Yt%d" % b], dma=("Yt", b))
            S.barrier()
        A.reset(rt_mark)

        if upto >= 6:
            Wg, Wp, gpostb = p6w
            NB6 = 3
            h5 = [A.alloc([1024]) for _ in range(NB6)]
            y0 = [A.alloc([1024]) for _ in range(NB6)]
            y1 = [A.alloc([1024]) for _ in range(NB6)]
            xpb = [A.alloc([1024], BF16) for _ in range(NB6)]
            xpT = [A.alloc([8, 128], BF16) for _ in range(NB6)]
            pt5 = [A.alloc([256]) for _ in range(NB6)]
            pb5 = [A.alloc([256], BF16) for _ in range(NB6)]
            ppT = [A.alloc([2, 128], BF16) for _ in range(NB6)]
            gt5 = [A.alloc([1024]) for _ in range(NB6)]
            pr5 = [A.alloc([1024]) for _ in range(NB6)]
            junk5 = A.alloc([1024])
            ss5 = A.alloc([2]); rstd5 = A.alloc([2]); ss6 = A.alloc([2]); rstd6 = A.alloc([2])
            for jt in range(32):
                b = jt % NB6
                rows = slice(jt * 128, (jt + 1) * 128)
                S.op("sp", lambda e, b=b, rows=rows: e.dma_start(out=h5[b], in_=Hd[rows, :]), w=["h5%d" % b], dma=("h5", b))
                S.op("sp", lambda e, b=b, rows=rows: e.dma_start(out=pt5[b], in_=pin[rows, :]), w=["pt5%d" % b], dma=("pt5", b))
                for sl, yy in enumerate((y0, y1)):
                    S.op("pool", lambda e, sl=sl, yy=yy, b=b, jt=jt: e.indirect_dma_start(
                        out=yy[b], out_offset=None, in_=Ys[:, :],
                        in_offset=bass.IndirectOffsetOnAxis(ap=pos_i[:, sl, jt:jt + 1], axis=0)),
                        w=["y%d%d" % (sl, b)], dma=("yg", sl, b))
                S.op("dve", lambda e, b=b, jt=jt: e.scalar_tensor_tensor(out=h5[b], in0=y0[b], scalar=gates[:, 0, jt:jt + 1], in1=h5[b], op0=ALU.mult, op1=ALU.add),
                     r=["y0%d" % b, "h5%d" % b], w=["h5%d" % b])
                S.op("dve", lambda e, b=b, jt=jt: e.scalar_tensor_tensor(out=h5[b], in0=y1[b], scalar=gates[:, 1, jt:jt + 1], in1=h5[b], op0=ALU.mult, op1=ALU.add),
                     r=["y1%d" % b, "h5%d" % b], w=["h5%d" % b])
                S.op("act", lambda e, b=b: e.activation(out=junk5, in_=h5[b], func=AF.Square, accum_out=ss5[:, 0:1]),
                     r=["h5%d" % b], w=["junk5", "ss5"], c=1.05)
                S.op("act", lambda e: e.activation(out=rstd5[:, 0:1], in_=ss5[:, 0:1], func=AF.Sqrt, scale=1.0 / 1024, bias=epst[:, 0:1]),
                     r=["ss5", "epst"], w=["rstd5"])
                S.op("dve", lambda e: e.reciprocal(out=rstd5[:, 0:1], in_=rstd5[:, 0:1]), r=["rstd5"], w=["rstd5"])
                S.op("dve", lambda e, b=b: e.tensor_scalar_mul(out=xpb[b], in0=h5[b], scalar1=rstd5[:, 0:1]),
                     r=["h5%d" % b, "rstd5"], w=["xpb%d" % b], c=0.7)
                ptr5 = RS(PB(6), 8, 128)
                for c in range(8):
                    S.op("pe", lambda e, c=c, b=b: e.transpose(out=ptr5[:, c, :], in_=xpb[b][:, c * 128:(c + 1) * 128], identity=identb),
                         r=["xpb%d" % b, "identb"], w=["ptr5"], c=0.11)
                S.op("act", lambda e, b=b: e.activation(out=xpT[b], in_=ptr5, func=AF.Copy), r=["ptr5"], w=["xpT%d" % b])
                S.op("act", lambda e, b=b: e.activation(out=pb5[b], in_=pt5[b], func=AF.Copy), r=["pt5%d" % b], w=["pb5%d" % b])
                ptp = RS(PB(7)[:, 0:256], 2, 128)
                for c in range(2):
                    S.op("pe", lambda e, c=c, b=b: e.transpose(out=ptp[:, c, :], in_=pb5[b][:, c * 128:(c + 1) * 128], identity=identb),
                         r=["pb5%d" % b, "identb"], w=["ptp"], c=0.11)
                S.op("dve", lambda e, b=b: e.tensor_copy(out=ppT[b], in_=ptp), r=["ptp"], w=["ppT%d" % b])
                pg_ = banks[0:2] if jt % 2 == 0 else banks[2:4]
                for half in range(2):
                    for c in range(8):
                        S.op("pe", lambda e, half=half, c=c, b=b, pg_=pg_: e.matmul(
                            pg_[half][:], lhsT=xpT[b][:, c, :], rhs=Wg[:, c, half * 512:(half + 1) * 512], start=(c == 0), stop=(c == 7)),
                            r=["xpT%d" % b] + pw_keys["Wg"], w=["pg%d%d" % (jt % 2, half)])
                    S.op("act", lambda e, half=half, b=b, pg_=pg_: e.activation(out=gt5[b][:, half * 512:(half + 1) * 512], in_=pg_[half][:], func=AF.Sigmoid),
                         r=["pg%d%d" % (jt % 2, half)], w=["gt5%d" % b])
                for half in range(2):
                    for c in range(2):
                        S.op("pe", lambda e, half=half, c=c, b=b: e.matmul(
                            banks[4 + half][:], lhsT=ppT[b][:, c, :], rhs=Wp[:, c, half * 512:(half + 1) * 512], start=(c == 0), stop=(c == 1)),
                            r=["ppT%d" % b] + pw_keys["Wp"], w=["pp%d" % half])
                    S.op("act", lambda e, half=half: e.activation(out=junk5[:, 0:512], in_=banks[4 + half][:], func=AF.Square, accum_out=ss6[:, half:half + 1]),
                         r=["pp%d" % half], w=["junk5", "ss6"])
                S.op("dve", lambda e: e.tensor_tensor(out=ss6[:, 0:1], in0=ss6[:, 0:1], in1=ss6[:, 1:2], op=ALU.add), r=["ss6"], w=["ss6"])
                S.op("act", lambda e: e.activation(out=rstd6[:, 0:1], in_=ss6[:, 0:1], func=AF.Sqrt, scale=1.0 / 1024, bias=epst[:, 0:1]),
                     r=["ss6", "epst"], w=["rstd6"])
                S.op("dve", lambda e: e.reciprocal(out=rstd6[:, 0:1], in_=rstd6[:, 0:1]), r=["rstd6"], w=["rstd6"])
                for half in range(2):
                    hs = slice(half * 512, (half + 1) * 512)
                    S.op("dve", lambda e, half=half, hs=hs, b=b: e.scalar_tensor_tensor(out=pr5[b][:, hs], in0=banks[4 + half][:], scalar=rstd6[:, 0:1], in1=gpostb[:, hs], op0=ALU.mult, op1=ALU.mult),
                         r=["pp%d" % half, "rstd6", "gpostb"], w=["pr5%d" % b])
                S.op("dve", lambda e, b=b: e.tensor_tensor(out=pr5[b], in0=pr5[b], in1=gt5[b], op=ALU.mult),
                     r=["pr5%d" % b, "gt5%d" % b], w=["pr5%d" % b], c=0.7)
                S.op("dve", lambda e, b=b: e.tensor_tensor(out=pr5[b], in0=pr5[b], in1=h5[b], op=ALU.add),
                     r=["pr5%d" % b, "h5%d" % b], w=["pr5%d" % b], c=0.7)
                fin.append(S.op("sp", lambda e, b=b, rows=rows: e.dma_start(out=out[rows, :], in_=pr5[b]),
                                r=["pr5%d" % b], dma=("out", b)))
        else:
            z = A.alloc([1024])
            S.op("pool", lambda e: e.memset(z, 0.0), w=["z"])
            fin.append(S.op("sp", lambda e: e.dma_start(out=out[0:128, :], in_=z), r=["z"], dma=("out", 0)))
        S.emit(final_wait_ops=fin)
    return nc


def _consts():
    k = np.arange(128)[:, None]
    q = np.arange(128)[None, :]
    prev = (q <= k).astype(np.float32)
    cur = (q >= k).astype(np.float32)
    mask4 = (np.concatenate([prev, cur, prev, cur], axis=1) - 1.0) * 240000.0
    ident = np.eye(128, dtype=np.float32)
    blk64 = np.zeros((128, 128), np.float32)
    blk64[:64, :64] = 1.0 / 64
    blk64[64:, 64:] = 1.0 / 64
    wa = np.zeros((128, 128), np.float32)
    wa[:64, :64] = 1.0 / 64
    wa[64:, :64] = EPS / 64
    wb = np.zeros((128, 128), np.float32)
    wb[64:, 64:] = 1.0 / 64
    wb[:64, 64:] = EPS / 64
    triu = (k < q).astype(np.float32)
    thr = np.broadcast_to((np.arange(17, dtype=np.float32) + 1) * TS, (128, 17)).copy()
    tstart = np.broadcast_to(np.arange(NT, dtype=np.float32) * TS, (128, NT)).copy()
    pidx = np.arange(128, dtype=np.float32).reshape(128, 1)
    return dict(ident=ident, mask4=mask4, blk64=blk64, wreda=wa, wredb=wb, triu=triu, thr=thr, tstart=tstart, pidx=pidx)


def _pc(v, nch):
    return np.ascontiguousarray(np.asarray(v, np.float32).reshape(nch, 128).T)


def _bc(v):
    v = np.asarray(v, np.float32).reshape(1, -1)
    return np.ascontiguousarray(np.broadcast_to(v, (128, v.shape[1])))


def make_in_maps(x, p, g_mix, w_in, q_norm, k_norm, conv_w, g_attn_out, g_conv_out, w_out, g_ffn,
                 w_router_group, b_router_group, w_router_expert, b_router_expert, w1, w3, w2,
                 g_ple, w_ple_gate, w_ple_proj, g_ple_post):
    f = lambda a: np.ascontiguousarray(np.asarray(a, np.float32))
    x = f(x); p = f(p)
    cs = _consts()
    shared = dict(cs)
    shared["gmix"] = _pc(g_mix[0], 8)
    shared["gq_b"] = _bc(np.tile(f(q_norm[0]), 8))
    shared["gk_b"] = _bc(np.tile(f(k_norm[0]), 8))
    shared["cw"] = np.ascontiguousarray(f(conv_w[0]).reshape(3, 4, 128).transpose(2, 1, 0).reshape(128, 12))
    shared["gout_pc"] = _pc(np.concatenate([f(g_attn_out[0]), f(g_conv_out[0])]), 8)
    shared["gffn_b"] = _bc(g_ffn[0])
    shared["gffn_pc"] = _pc(g_ffn[0], 8)
    shared["gple_pc"] = _pc(g_ple[0], 8)
    shared["gpost_b"] = _bc(g_ple_post[0])
    shared["rbias_b"] = _bc(np.concatenate([f(b_router_group[0]), f(b_router_expert[0])]))
    shared["w_in"] = f(w_in[0])
    shared["w_out"] = f(w_out[0])
    shared["w_r"] = np.ascontiguousarray(np.concatenate([f(w_router_group[0]), f(w_router_expert[0])], axis=1))
    shared["w1"] = f(w1[0]).reshape(16 * 128, 8 * 512)
    shared["w3"] = f(w3[0]).reshape(16 * 128, 8 * 512)
    shared["w2"] = f(w2[0]).reshape(16 * 128, 4 * 1024)
    shared["w_pg"] = f(w_ple_gate[0])
    shared["w_pp"] = f(w_ple_proj[0])
    half_inv = 500000.0 ** (-np.arange(0, 16, 2, dtype=np.float64) / 16)
    maps = []
    for c in range(8):
        b, hf = divmod(c, 2)
        t0 = hf * NTOK - NHALO
        xin = np.zeros((NALL, 1024), np.float32)
        lo = max(t0, 0)
        xin[lo - t0:, :] = x[b, lo:t0 + NALL, :]
        tau = np.arange(NALL)
        exists = ((tau + t0) >= 0).astype(np.float32)
        ang = (tau + t0).astype(np.float64)[:, None] * half_inv[None, :]
        lay = lambda a: np.ascontiguousarray(a.reshape(48, 128, -1).transpose(1, 0, 2).reshape(128, -1).astype(np.float32))
        m = dict(shared)
        m["xin"] = xin
        m["pin"] = np.ascontiguousarray(p[0, b, hf * NTOK:(hf + 1) * NTOK, :])
        m["ex"] = lay(exists[:, None])
        m["cos"] = lay(np.cos(ang))
        m["sin"] = lay(np.sin(ang))
        maps.append(m)
    return maps


_NC_CACHE = {}


def kernel(**inputs):
    if "nc" not in _NC_CACHE:
        _NC_CACHE["nc"] = build_nc()
    nc = _NC_CACHE["nc"]
    maps = make_in_maps(**inputs)
    res = run_bass_kernel_spmd(nc, maps, core_ids=list(range(8)))
    outs = [np.asarray(r["out"], np.float32) for r in res.results]
    full = np.zeros((4, 8192, 1024), np.float32)
    for c in range(8):
        b, hf = divmod(c, 2)
        full[b, hf * NTOK:(hf + 1) * NTOK, :] = outs[c]
    return full
```

```python
import contextlib
import numpy as np
import concourse.bass as bass
import concourse.mybir as mybir
from concourse.bass_utils import run_bass_kernel_spmd

F32 = mybir.dt.float32
BF16 = mybir.dt.bfloat16
I32 = mybir.dt.int32
ALU = mybir.AluOpType
AF = mybir.ActivationFunctionType
AX = mybir.AxisListType

NTOK = 4096
NHALO = 2048
NALL = NTOK + NHALO
TS = 512
NT = 32
EPS = 1e-6
P1_RANGE = range(48)
BRANCHES = (1, 4, 16)
P2_STOP = 4
MASK_ENG = 'dve'


SEG_T = []


class Sched:
    ENGS = ("pe", "act", "dve", "pool", "sp")
    DEF_COST = {"pe": 0.25, "act": 0.7, "dve": 0.55, "pool": 1.3, "sp": 0.15}
    DMA_LAT = 3.0
    WINDOW = 96

    def __init__(self, nc):
        self.nc = nc
        self.ops = []
        self.last_w = {}
        self.readers = {}
        self.dma_eng = {}
        self.seg = 0

    def op(self, eng, fn, r=(), w=(), dma=None, c=None):
        oid = len(self.ops)
        deps = set()
        for k in r:
            if k in self.last_w:
                deps.add(self.last_w[k])
        for k in w:
            if k in self.last_w:
                deps.add(self.last_w[k])
            for rd in self.readers.get(k, ()):
                deps.add(rd)
        for k in r:
            self.readers.setdefault(k, []).append(oid)
        for k in w:
            self.last_w[k] = oid
            self.readers[k] = []
        if dma is not None:
            assert self.dma_eng.setdefault(dma, eng) == eng
        self.ops.append(dict(eng=eng, fn=fn, deps=deps, dma=dma, sig=False, idx=None, seg=self.seg, c=c))
        return oid

    def barrier(self):
        self.seg += 1

    def schedule(self):
        ops = self.ops
        n = len(ops)
        ENGS = self.ENGS
        finish = [0.0] * n
        eng_free = {e: 0.0 for e in ENGS}
        order = {e: [] for e in ENGS}
        last_eng = {}
        last_dma = {}
        byseg = [[] for _ in range(self.seg + 1)]
        for i, o in enumerate(ops):
            byseg[o["seg"]].append(i)
        tseg = 0.0
        W = self.WINDOW
        for s, ids in enumerate(byseg):
            if not ids:
                continue
            bar = set(last_eng.values()) | set(last_dma.values())
            if s > 0:
                for i in ids:
                    ops[i]["deps"] |= bar
            idset = set(ids)
            users = {i: [] for i in ids}
            nun = {}
            ready = {}
            for i in ids:
                k = 0
                for d in ops[i]["deps"]:
                    if d in idset:
                        users[d].append(i)
                        k += 1
                nun[i] = k
                ready[i] = tseg
            pending = {e: [i for i in ids if ops[i]["eng"] == e] for e in ENGS}
            for e in ENGS:
                eng_free[e] = max(eng_free[e], tseg)
            remaining = len(ids)
            while remaining:
                best = None
                for e in ENGS:
                    ef = eng_free[e]
                    cand = None
                    cnt = 0
                    for i in pending[e]:
                        cnt += 1
                        if cnt > W:
                            break
                        if nun[i]:
                            continue
                        rt = ready[i]
                        if rt <= ef:
                            cand = (ef, i)
                            break
                        if cand is None or rt < cand[0]:
                            cand = (rt, i)
                    if cand is not None and (best is None or cand[0] < best[0]):
                        best = (cand[0], e, cand[1])
                t0, e, i = best
                o = ops[i]
                if o["dma"] is not None:
                    issue = 1.1 if e == "pool" else 0.15
                    fin = t0 + issue + (o["c"] if o["c"] is not None else self.DMA_LAT)
                    last_dma[o["dma"]] = i
                else:
                    issue = o["c"] if o["c"] is not None else self.DEF_COST[e]
                    fin = t0 + issue + 0.1
                    last_eng[e] = i
                eng_free[e] = t0 + issue
                finish[i] = fin
                pending[e].remove(i)
                order[e].append(i)
                remaining -= 1
                for u in users[i]:
                    nun[u] -= 1
                    if ready[u] < fin:
                        ready[u] = fin
            tseg = max(finish[i] for i in ids)
            tseg = max([tseg] + list(eng_free.values()))
            SEG_T.append(round(tseg, 1))
        self.est_total = tseg
        return order

    def emit(self, final_wait_ops=()):
        nc = self.nc
        ops = self.ops
        order = self.schedule()
        dcnt = {}
        for e in self.ENGS:
            for i in order[e]:
                o = ops[i]
                if o["dma"] is not None:
                    c = dcnt.get(o["dma"], 0) + 1
                    dcnt[o["dma"]] = c
                    o["dcount"] = c
        for o in ops:
            for d in o["deps"]:
                p = ops[d]
                if p["dma"] is None:
                    if p["eng"] == "pe" and o["eng"] == "pe" and o["dma"] is None:
                        continue
                    p["sig"] = True
        for d in final_wait_ops:
            if ops[d]["dma"] is None:
                ops[d]["sig"] = True
        for e in self.ENGS:
            k = 0
            for i in order[e]:
                o = ops[i]
                if o["dma"] is None and o["sig"]:
                    k += 1
                    o["idx"] = k
        with contextlib.ExitStack() as es:
            esem = {e: es.enter_context(nc.semaphore("S_" + e)) for e in self.ENGS}
            dsem = {}
            for i, k in enumerate(dcnt):
                dsem[k] = es.enter_context(nc.semaphore("D%d" % i))
            block = es.enter_context(nc.Block())

            def run(engname, engobj):
                waited = {}
                for oi in order[engname]:
                    o = ops[oi]
                    need = {}
                    for d in sorted(o["deps"]):
                        p = ops[d]
                        if p["dma"] is not None:
                            s, v = dsem[p["dma"]], 16 * p["dcount"]
                        else:
                            if p["idx"] is None:
                                continue
                            if p["eng"] == "pe" and engname == "pe" and o["dma"] is None:
                                continue
                            s, v = esem[p["eng"]], p["idx"]
                        key = id(s)
                        if key not in need or need[key][1] < v:
                            need[key] = (s, v)
                    for key, (s, v) in need.items():
                        if waited.get(key, 0) >= v:
                            continue
                        waited[key] = v
                        engobj.wait_ge(s, v)
                    ins = o["fn"](engobj)
                    if o["dma"] is not None:
                        ins.then_inc(dsem[o["dma"]], 16)
                    elif o["sig"]:
                        ins.then_inc(esem[engname], 1)
                if engname == "sp":
                    for d in final_wait_ops:
                        p = ops[d]
                        if p["dma"] is not None:
                            engobj.wait_ge(dsem[p["dma"]], 16 * p["dcount"])
                        else:
                            engobj.wait_ge(esem[p["eng"]], p["idx"])

            @block.tensor
            def _(e):
                run("pe", e)

            @block.scalar
            def _(e):
                run("act", e)

            @block.vector
            def _(e):
                run("dve", e)

            @block.gpsimd
            def _(e):
                run("pool", e)

            @block.sync
            def _(e):
                run("sp", e)


def RS(ap, *dims):
    names = "abcdef"[:len(dims)]
    kw = {n: d for n, d in zip(names[:-1], dims[:-1])}
    return ap.rearrange("p (%s) -> p %s" % (" ".join(names), " ".join(names)), **kw)


class Arena:
    def __init__(self, t, nwords):
        self.t = t
        self.n = nwords
        self.n0 = nwords
        self.off = 0
        self.hw = 0

    def alloc(self, dims, dtype=F32):
        n = int(np.prod(dims))
        sz = {F32: 4, BF16: 2, I32: 4}[dtype]
        words = (n * sz + 3) // 4
        words = (words + 7) // 8 * 8
        assert self.off + words <= self.n, ("arena overflow", self.off, words, self.n)
        ap = self.t[:, self.off:self.off + words]
        self.off += words
        self.hw = max(self.hw, self.off)
        if dtype != F32:
            ap = ap.bitcast(dtype)
        ap = ap[:, 0:n]
        if len(dims) > 1:
            ap = RS(ap, *dims)
        return ap

    def alloc_top(self, dims, dtype=F32):
        n = int(np.prod(dims))
        sz = {F32: 4, BF16: 2, I32: 4}[dtype]
        words = ((n * sz + 3) // 4 + 7) // 8 * 8
        self.n -= words
        assert self.off <= self.n, ("arena overflow (top)", self.off, words, self.n)
        ap = self.t[:, self.n:self.n + words]
        if dtype != F32:
            ap = ap.bitcast(dtype)
        ap = ap[:, 0:n]
        if len(dims) > 1:
            ap = RS(ap, *dims)
        return ap

    def free_top(self):
        self.n = self.n0

    def mark(self):
        return self.off

    def reset(self, m):
        self.off = m


def build_nc(upto=99, dbg=False):
    nc = bass.Bass("TRN2", target_bir_lowering=False)
    D = lambda n, s, dt=F32: nc.dram_tensor(n, s, dt, kind="ExternalInput").ap()
    xin = D("xin", [NALL, 1024])
    pin = D("pin", [NTOK, 256])
    exd = D("ex", [128, 48])
    cosd = D("cos", [128, 48 * 8])
    sind = D("sin", [128, 48 * 8])
    gmixd = D("gmix", [128, 8])
    gqd = D("gq_b", [128, 512])
    gkd = D("gk_b", [128, 512])
    cwd = D("cw", [128, 12])
    gaod = D("gout_pc", [128, 8])
    gffnbd = D("gffn_b", [128, 1024])
    gffnpc = D("gffn_pc", [128, 8])
    gplepc = D("gple_pc", [128, 8])
    gpostbd = D("gpost_b", [128, 1024])
    rbiasd = D("rbias_b", [128, 20])
    w_in = D("w_in", [1024, 3072])
    w_out = D("w_out", [1024, 1024])
    w_r = D("w_r", [1024, 20])
    w1 = D("w1", [16 * 128, 8 * 512])
    w3 = D("w3", [16 * 128, 8 * 512])
    w2 = D("w2", [16 * 128, 4 * 1024])
    w_pg = D("w_pg", [1024, 1024])
    w_pp = D("w_pp", [256, 1024])
    identd = D("ident", [128, 128])
    mask4d = D("mask4", [128, 512])
    blk64d = D("blk64", [128, 128])
    wredad = D("wreda", [128, 128])
    wredbd = D("wredb", [128, 128])
    triud = D("triu", [128, 128])
    thrd = D("thr", [128, 17])
    tstartd = D("tstart", [128, NT])
    pidxd = D("pidx", [128, 1])
    out = nc.dram_tensor("out", [NTOK, 1024], F32, kind="ExternalOutput").ap()

    I = lambda n, s, dt: nc.dram_tensor(n, s, dt, kind="Internal").ap()
    QT = I("QT", [4, 128, NTOK], BF16)
    KT = I("KT", [4, 128, NALL], BF16)
    Vs2 = I("Vs2", [4, NALL, 192], BF16)
    Vs1 = I("Vs1", [4, 128, 48 * 192], BF16)
    CT = I("CT", [4, 128, NTOK], BF16)
    AT = I("AT", [4, 128, NTOK], BF16)
    Hd = I("Hd", [NTOK, 1024], F32)
    Xs = I("Xs", [NT * TS, 1024], BF16)
    Ys = I("Ys", [NT * TS, 1024], F32)
    dbgo = {}
    if dbg:
        O = lambda n, s, dt=F32: nc.dram_tensor(n, s, dt, kind="ExternalOutput").ap()
        dbgo["gates"] = O("d_gates", [128, 64])
        dbgo["pos"] = O("d_pos", [128, 64], I32)
        dbgo["widx"] = O("d_widx", [128, NT], I32)
        dbgo["lg"] = O("d_lg", [128, 32 * 20])

    NW = 51 * 1024
    with contextlib.ExitStack() as es:
        arena_t = es.enter_context(nc.sbuf_tensor("arena", [128, NW], F32))
        banks = [es.enter_context(nc.psum_tensor("bank%d" % i, [128, 512], F32)) for i in range(8)]
        A = Arena(arena_t, NW)
        S = Sched(nc)
        fin = []
        pw_keys = {}

        def PB(i):
            return banks[i][:].bitcast(BF16)

        def ld(dst, src, key, eng="sp"):
            return S.op(eng, lambda e: e.dma_start(out=dst, in_=src), w=[key], dma="ldc")

        stg = A.alloc([4096])
        stg_b = A.alloc([4096])
        identf = A.alloc([128]); ld(identf, identd[:, :], "identf")
        identb = A.alloc([128], BF16)
        epst = A.alloc([1])
        S.op("pool", lambda e: e.memset(epst, EPS), w=["epst"])

        def const_bf(src_d, n, key):
            t = A.alloc([n], BF16)
            S.op("sp", lambda e: e.dma_start(out=stg[:, 0:n], in_=src_d), w=["stg0"], dma=("ld", "stg0"))
            S.op("dve", lambda e: e.tensor_copy(out=t, in_=stg[:, 0:n]), r=["stg0"], w=[key])
            return t

        mask4 = const_bf(mask4d[:, :], 512, "mask4")
        blk64 = const_bf(blk64d[:, :], 128, "blk64")
        wreda = const_bf(wredad[:, :], 128, "wreda")
        wredb = const_bf(wredbd[:, :], 128, "wredb")
        triu = const_bf(triud[:, :], 128, "triu")
        onesb = A.alloc([128], BF16)
        S.op("pool", lambda e: e.memset(onesb, 1.0), w=["onesb"])

        def const_f(src_d, dims, key):
            t = A.alloc(dims)
            n = int(np.prod(dims))
            flat = t if len(dims) == 1 else t.rearrange("p a b -> p (a b)")
            ld(flat, src_d, key)
            return t

        ext = const_f(exd[:, :], [48], "ex")
        cost = const_f(cosd[:, :], [48, 8], "cos")
        sint = const_f(sind[:, :], [48, 8], "sin")
        gmix = const_f(gmixd[:, :], [8], "gmix")
        gao = const_f(gaod[:, :], [8], "gao")
        gffnpc_t = const_f(gffnpc[:, :], [8], "gffnpc")
        gplepc_t = const_f(gplepc[:, :], [8], "gplepc")
        cw = const_f(cwd[:, :], [4, 3], "cw")
        rbias = const_f(rbiasd[:, :], [20], "rbias")
        thr = const_f(thrd[:, :], [17], "thr")
        tstart = const_f(tstartd[:, :], [NT], "tstart")
        pidx = const_f(pidxd[:, :], [1], "pidx")
        lgall = A.alloc([32, 20])
        gates = A.alloc([2, 32])
        pos_i = A.alloc([2, 32], I32)
        widx_i = A.alloc([NT], I32)
        base_mark = A.mark()
        S.barrier()
        S.op("dve", lambda e: e.tensor_copy(out=identb, in_=identf), r=["identf"], w=["identb"])

        stg2 = [stg, stg_b]
        stg_n = [0]

        def prep_weight(dst_bf, src_rows_ap, nchunk, ncols, gain_pc, key, engs=("dve", "act")):
            si = stg_n[0] % 2
            stg_n[0] += 1
            sk = "stg%d" % si
            sv = RS(stg2[si][:, 0:nchunk * ncols], nchunk, ncols)
            S.op("sp", lambda e: e.dma_start(out=sv, in_=src_rows_ap), w=[sk], dma=("ld", sk), c=14.0)
            for c in range(nchunk):
                en = engs[c % len(engs)]
                kc = "%s.%d.%d" % (key, stg_n[0], c)
                wk = [key, kc]
                if en == "act":
                    if gain_pc is not None:
                        S.op("act", lambda e, c=c: e.activation(out=dst_bf[:, c, :], in_=sv[:, c, :], func=AF.Copy, scale=gain_pc[:, c:c + 1]),
                             r=[sk], w=[kc], c=ncols / 512 * 0.6)
                    else:
                        S.op("act", lambda e, c=c: e.activation(out=dst_bf[:, c, :], in_=sv[:, c, :], func=AF.Copy), r=[sk], w=[kc], c=ncols / 512 * 0.6)
                else:
                    if gain_pc is not None:
                        S.op(en, lambda e, c=c: e.tensor_scalar_mul(out=dst_bf[:, c, :], in0=sv[:, c, :], scalar1=gain_pc[:, c:c + 1]),
                             r=[sk], w=[kc], c=ncols / 512 * 0.5)
                    else:
                        S.op(en, lambda e, c=c: e.tensor_copy(out=dst_bf[:, c, :], in_=sv[:, c, :]), r=[sk], w=[kc], c=ncols / 512 * 0.5)
            pw_keys.setdefault(key, []).extend("%s.%d.%d" % (key, stg_n[0], c) for c in range(nchunk))

        def prep_p3():
            Wout = A.alloc_top([8, 1024], BF16)
            Wr = A.alloc_top([8, 20])
            gffnb = A.alloc_top([1024])
            for nb in range(2):
                prep_weight(Wout[:, :, nb * 512:(nb + 1) * 512],
                            w_out[:, nb * 512:(nb + 1) * 512].rearrange("(c p) f -> p c f", p=128), 8, 512, gao, "Wout")
            S.op("sp", lambda e: e.dma_start(out=RS(stg[:, 0:160], 8, 20), in_=w_r.rearrange("(c p) f -> p c f", p=128)),
                 w=["stg0"], dma=("ld", "stg0"))
            for c in range(8):
                S.op("dve", lambda e, c=c: e.tensor_scalar_mul(out=Wr[:, c, :], in0=RS(stg[:, 0:160], 8, 20)[:, c, :], scalar1=gffnpc_t[:, c:c + 1]),
                     r=["stg0", "gffnpc"], w=["Wr"], c=0.1)
            ld(gffnb, gffnbd[:, :], "gffnb")
            return Wout, Wr, gffnb

        def prep_p6():
            Wg = A.alloc_top([8, 1024], BF16)
            Wp = A.alloc_top([2, 1024], BF16)
            gpostb = A.alloc_top([1024])
            for nb in range(2):
                prep_weight(Wg[:, :, nb * 512:(nb + 1) * 512],
                            w_pg[:, nb * 512:(nb + 1) * 512].rearrange("(c p) f -> p c f", p=128), 8, 512, gplepc_t, "Wg")
            prep_weight(Wp, w_pp.rearrange("(c p) f -> p c f", p=128), 2, 1024, None, "Wp")
            ld(gpostb, gpostbd[:, :], "gpostb")
            return Wg, Wp, gpostb

        if upto >= 1:
            Win = A.alloc([8, 3072], BF16)
            win_id = {}
            for nb in (1, 2, 0, 3, 4, 5):
                prep_weight(Win[:, :, nb * 512:(nb + 1) * 512],
                            w_in[:, nb * 512:(nb + 1) * 512].rearrange("(c p) f -> p c f", p=128),
                            8, 512, gmix, "Win")
                win_id[nb] = stg_n[0]
            gqb = const_f(gqd[:, :], [512], "gqb")
            gkb = const_f(gkd[:, :], [512], "gkb")
            xt = [A.alloc([1024]) for _ in range(2)]
            junk = A.alloc([1024])
            junk2 = [A.alloc([512]) for _ in range(2)]
            ss = A.alloc([2]); rstd = A.alloc([2])
            xb = [A.alloc([1024], BF16) for _ in range(2)]
            xnT = [A.alloc([8, 512], BF16) for _ in range(2)]
            qn = [A.alloc([512]) for _ in range(2)]
            ss8 = [A.alloc([8]) for _ in range(2)]
            r8 = [A.alloc([8, 1]) for _ in range(2)]
            rt = [[A.alloc([8, 8]) for _ in range(4)] for _ in range(2)]
            qb = [A.alloc([512], BF16) for _ in range(2)]
            qTs = A.alloc([4, 512], BF16)
            kTs = A.alloc([4, 512], BF16)
            vst = [A.alloc([4, 3, 64], BF16) for _ in range(2)]
            chs = A.alloc([512])
            ut = A.alloc([4, 514])
            yt = A.alloc([512])
            ot = A.alloc([512])
            sqb = A.alloc([512], BF16)
            rsq = A.alloc([512])
            cst = A.alloc([4, 512], BF16)
            S.op("pool", lambda e: e.memset(ut.rearrange("p a b -> p (a b)"), 0.0), w=["ut"])
            B_TR, B_Q, B_K, B_V, B_TQ, B_CB, B_CC, B_CH = range(8)
            ptr = RS(PB(B_TR), 8, 128)
            ptq = RS(PB(B_TQ)[:, 0:512], 4, 128)

            def qk_proc(pz, gain_b, j, stage, slot, w):
                jk = junk2[w]; q_n = qn[w]; q_b = qb[w]
                S.op("act", lambda e: e.activation(out=jk, in_=pz, func=AF.Square), r=["pz%d" % w], w=["jk%d" % w])
                S.op("dve", lambda e: e.reduce_sum(out=ss8[w], in_=RS(jk, 8, 64), axis=AX.X), r=["jk%d" % w], w=["ss8%d" % w])
                r8f = r8[w].rearrange("p a b -> p (a b)")
                S.op("act", lambda e: e.activation(out=r8f, in_=ss8[w], func=AF.Ln, scale=1.0 / 64, bias=epst[:, 0:1]),
                     r=["ss8%d" % w, "epst"], w=["r8%d" % w], c=0.2)
                S.op("act", lambda e: e.activation(out=r8f, in_=r8f, func=AF.Exp, scale=-0.5), r=["r8%d" % w], w=["r8%d" % w], c=0.2)
                S.op("dve", lambda e: e.tensor_tensor(out=RS(q_n, 8, 64), in0=RS(pz, 8, 64), in1=r8[w].to_broadcast([128, 8, 64]), op=ALU.mult),
                     r=["pz%d" % w, "r8%d" % w], w=["qn%d" % w])
                S.op("dve", lambda e: e.tensor_tensor(out=q_n, in0=q_n, in1=gain_b, op=ALU.mult),
                     r=["qn%d" % w, "gqb", "gkb"], w=["qn%d" % w], c=0.45)
                q3 = RS(q_n, 8, 64); b3 = RS(q_b, 8, 64)
                cb = cost[:, j:j + 1, :].to_broadcast([128, 8, 8]); sb = sint[:, j:j + 1, :].to_broadcast([128, 8, 8])
                t1, t2, t3, t4 = rt[w]
                kq = ["qn%d" % w, "cos", "sin"]
                S.op("dve", lambda e: e.tensor_tensor(out=t1, in0=q3[:, :, 0:8], in1=cb, op=ALU.mult), r=kq, w=["t1%d" % w])
                S.op("pool", lambda e: e.tensor_tensor(out=t2, in0=q3[:, :, 8:16], in1=sb, op=ALU.mult), r=kq, w=["t2%d" % w])
                S.op("dve", lambda e: e.tensor_tensor(out=t3, in0=q3[:, :, 8:16], in1=cb, op=ALU.mult), r=kq, w=["t3%d" % w])
                S.op("pool", lambda e: e.tensor_tensor(out=t4, in0=q3[:, :, 0:8], in1=sb, op=ALU.mult), r=kq, w=["t4%d" % w])
                S.op("dve", lambda e: e.tensor_tensor(out=b3[:, :, 0:8], in0=t1, in1=t2, op=ALU.subtract),
                     r=["t1%d" % w, "t2%d" % w], w=["qb%d" % w])
                S.op("dve", lambda e: e.tensor_tensor(out=b3[:, :, 8:16], in0=t3, in1=t4, op=ALU.add),
                     r=["t3%d" % w, "t4%d" % w], w=["qb%d" % w])
                S.op("act", lambda e: e.activation(out=b3[:, :, 16:64], in_=q3[:, :, 16:64], func=AF.Copy), r=["qn%d" % w], w=["qb%d" % w], c=0.5)
                for pr in range(4):
                    S.op("pe", lambda e, pr=pr: e.transpose(out=ptq[:, pr, :], in_=q_b[:, pr * 128:(pr + 1) * 128], identity=identb),
                         r=["qb%d" % w, "identb"], w=["ptq"], c=0.11)
                S.op("act", lambda e: e.activation(out=stage[:, :, slot * 128:(slot + 1) * 128], in_=ptq, func=AF.Copy),
                     r=["ptq"], w=["stage%d" % w])

            for j in P1_RANGE:
                g, s = divmod(j, 4)
                own = j >= 16
                xi = j % 2
                S.op("sp", lambda e, j=j, xi=xi: e.dma_start(out=xt[xi], in_=xin[j * 128:(j + 1) * 128, :]),
                     w=["xt%d" % xi], dma=("xt", xi))
                S.op("act", lambda e, xi=xi: e.activation(out=junk, in_=xt[xi], func=AF.Square, accum_out=ss[:, 0:1]),
                     r=["xt%d" % xi], w=["junk", "ss"], c=1.05)
                S.op("act", lambda e: e.activation(out=rstd[:, 0:1], in_=ss[:, 0:1], func=AF.Ln, scale=1.0 / 1024, bias=epst[:, 0:1]),
                     r=["ss", "epst"], w=["rstd"], c=0.2)
                S.op("act", lambda e: e.activation(out=rstd[:, 0:1], in_=rstd[:, 0:1], func=AF.Exp, scale=-0.5), r=["rstd"], w=["rstd"], c=0.2)
                S.op("dve", lambda e, xi=xi: e.tensor_scalar_mul(out=xb[xi], in0=xt[xi], scalar1=rstd[:, 0:1]),
                     r=["xt%d" % xi, "rstd"], w=["xb%d" % xi], c=0.7)
                for c in range(8):
                    S.op("pe", lambda e, c=c, xi=xi: e.transpose(out=ptr[:, c, :], in_=xb[xi][:, c * 128:(c + 1) * 128], identity=identb),
                         r=["xb%d" % xi, "identb"], w=["ptr"], c=0.11)
                xg = xnT[g % 2]
                S.op("act", lambda e, xg=xg, s=s: e.activation(out=xg[:, :, s * 128:(s + 1) * 128], in_=ptr, func=AF.Copy),
                     r=["ptr"], w=["xnT%d" % (g % 2)])
                nbs = ([0] if own else []) + [1, 2]
                for c in range(8):
                    for nb in nbs:
                        S.op("pe", lambda e, c=c, nb=nb, xg=xg, s=s: e.matmul(
                            banks[B_Q + nb][:], lhsT=xg[:, c, s * 128:(s + 1) * 128], rhs=Win[:, c, nb * 512:(nb + 1) * 512],
                            start=(c == 0), stop=(c == 7)),
                            r=["xnT%d" % (g % 2), "Win.%d.%d" % (win_id[nb], c)], w=["pz%d" % nb if nb < 2 else "pv"])
                if own:
                    qk_proc(banks[B_Q][:], gqb, j, qTs, s, 0)
                qk_proc(banks[B_K][:], gkb, j, kTs, s, 1)
                vi = j % 2
                v4 = RS(banks[B_V][:], 4, 2, 64)
                S.op("act", lambda e, vi=vi, v4=v4: e.activation(out=vst[vi][:, :, 0:3:2, :], in_=v4, func=AF.Copy),
                     r=["pv"], w=["vst%d" % vi])
                S.op("pool", lambda e, vi=vi, j=j: e.tensor_copy(out=vst[vi][:, :, 1, :], in_=RS(ext, 48, 1)[:, j:j + 1, :].to_broadcast([128, 4, 64])),
                     r=["ex"], w=["vst%d" % vi])
                S.op("sp", lambda e, vi=vi, j=j: e.dma_start(out=Vs2[:, j * 128:(j + 1) * 128, :].rearrange("a p c -> p a c"),
                                                             in_=vst[vi].rearrange("p a b c -> p a (b c)")),
                     r=["vst%d" % vi], dma=("vst", vi))
                S.op("sp", lambda e, vi=vi, j=j: e.dma_start(out=Vs1[:, :, j * 192:(j + 1) * 192].rearrange("a p c -> p a c"),
                                                             in_=vst[vi].rearrange("p a b c -> p a (b c)")),
                     r=["vst%d" % vi], dma=("vst", vi))
                if s == 3:
                    if own:
                        S.op("sp", lambda e, g=g: e.dma_start(out=QT[:, :, (g - 4) * 512:(g - 3) * 512].rearrange("a p t -> p a t"), in_=qTs),
                             r=["stage0"], dma=("qTs",))
                    S.op("sp", lambda e, g=g: e.dma_start(out=KT[:, :, g * 512:(g + 1) * 512].rearrange("a p t -> p a t"), in_=kTs),
                         r=["stage1"], dma=("kTs",))
                    if g >= 3:
                        for ci in range(4):
                            for bi, (bk, coff) in enumerate(((B_CB, 1536), (B_CC, 2048), (B_CH, 2560))):
                                col = coff + ci * 128
                                for c in range(8):
                                    S.op("pe", lambda e, c=c, bk=bk, col=col, xg=xg: e.matmul(
                                        banks[bk][:], lhsT=Win[:, c, col:col + 128], rhs=xg[:, c, :], start=(c == 0), stop=(c == 7)),
                                        r=["xnT%d" % (g % 2), "Win.%d.%d" % (win_id[3 + bi], c)], w=["pc%d" % bi])
                            S.op("act", lambda e: e.activation(out=chs, in_=banks[B_CH][:], func=AF.Copy), r=["pc2"], w=["chs"])
                            S.op("dve", lambda e, ci=ci: e.tensor_tensor(out=ut[:, ci, 2:514], in0=banks[B_CC][:], in1=chs, op=ALU.mult),
                                 r=["pc1", "chs"], w=["ut"])
                            if g >= 4:
                                S.op("dve", lambda e, ci=ci: e.tensor_scalar_mul(out=yt, in0=ut[:, ci, 2:514], scalar1=cw[:, ci, 2:3]),
                                     r=["ut", "cw"], w=["yt"])
                                S.op("dve", lambda e, ci=ci: e.scalar_tensor_tensor(out=yt, in0=ut[:, ci, 1:513], scalar=cw[:, ci, 1:2], in1=yt, op0=ALU.mult, op1=ALU.add),
                                     r=["ut", "cw", "yt"], w=["yt"])
                                S.op("dve", lambda e, ci=ci: e.scalar_tensor_tensor(out=yt, in0=ut[:, ci, 0:512], scalar=cw[:, ci, 0:1], in1=yt, op0=ALU.mult, op1=ALU.add),
                                     r=["ut", "cw", "yt"], w=["yt"])
                                S.op("dve", lambda e: e.tensor_tensor(out=ot, in0=yt, in1=banks[B_CB][:], op=ALU.mult),
                                     r=["yt", "pc0"], w=["ot"])
                                S.op("act", lambda e: e.activation(out=sqb, in_=ot, func=AF.Square), r=["ot"], w=["sqb"])
                                S.op("pe", lambda e: e.matmul(banks[B_CC][:], lhsT=blk64, rhs=sqb, start=True, stop=True),
                                     r=["sqb", "blk64", "ut"], w=["pc1"])
                                S.op("act", lambda e: e.activation(out=rsq, in_=banks[B_CC][:], func=AF.Ln, bias=epst[:, 0:1]),
                                     r=["pc1", "epst"], w=["rsq"], c=0.6)
                                S.op("act", lambda e: e.activation(out=rsq, in_=rsq, func=AF.Exp, scale=-0.5), r=["rsq"], w=["rsq"], c=0.6)
                                S.op("pool", lambda e, ci=ci: e.tensor_tensor(out=cst[:, ci, :], in0=ot, in1=rsq, op=ALU.mult),
                                     r=["ot", "rsq"], w=["cst"])
                            S.op("pool", lambda e, ci=ci: e.tensor_copy(out=ut[:, ci, 0:2], in_=ut[:, ci, 512:514]), r=["ut"], w=["ut"])
                        if g >= 4:
                            S.op("sp", lambda e, g=g: e.dma_start(out=CT[:, :, (g - 4) * 512:(g - 3) * 512].rearrange("a p t -> p a t"), in_=cst),
                                 r=["cst"], dma=("cst",))
            S.barrier()
        A.reset(base_mark)

        if upto >= 2:
            p3w = prep_p3() if upto >= 3 else None
            qTz = A.alloc([2, NTOK], BF16)
            kT = A.alloc([NALL], BF16)
            qTz16 = A.alloc([2, 16, NTOK // 16], BF16)
            kT16 = A.alloc([16, NALL // 16], BF16)
            S.op("pool", lambda e: e.memset(qTz[64:128, 0, :], 0.0), w=["qT"])
            S.op("pool", lambda e: e.memset(qTz[0:64, 1, :], 0.0), w=["qT"])
            Vd = [A.alloc([48 * 192], BF16) for _ in range(2)]
            acc = A.alloc([2, NTOK])
            Pt = [A.alloc([4, 128], BF16) for _ in range(4)]
            sq2 = [A.alloc([2, 512], BF16) for _ in range(2)]
            rs2 = A.alloc([512])
            ats = [A.alloc([512], BF16) for _ in range(2)]
            it = 0
            vcount = 0
            for pr in range(4):
                S.op("sp", lambda e, pr=pr: e.dma_start(out=qTz[0:64, 0, :], in_=QT[pr, 0:64, :]), w=["qT"], dma=("qT",))
                S.op("sp", lambda e, pr=pr: e.dma_start(out=qTz[64:128, 1, :], in_=QT[pr, 64:128, :]), w=["qT"], dma=("qT",))
                S.op("sp", lambda e, pr=pr: e.dma_start(out=kT, in_=KT[pr, :, :]), w=["kT"], dma=("kT",))
                perm_chunks = []
                if 16 in BRANCHES:
                    for hh in range(2):
                        for g4 in range(4):
                            rs_ = slice(g4 * 4, (g4 + 1) * 4)
                            perm_chunks.append(lambda hh=hh, g4=g4, rs_=rs_: S.op(
                                "dve", lambda e: e.tensor_copy(out=qTz16[:, hh, rs_, :],
                                                               in_=qTz[:, hh, :].rearrange("p (n r) -> p r n", r=16)[:, rs_, :]),
                                r=["qT"], w=["qT16.%d.%d" % (hh, g4)], c=1.2))
                    for g2 in range(8):
                        rs_ = slice(g2 * 2, (g2 + 1) * 2)
                        perm_chunks.append(lambda g2=g2, rs_=rs_: S.op(
                            "act", lambda e: e.activation(out=kT16[:, rs_, :], in_=kT.rearrange("p (n r) -> p r n", r=16)[:, rs_, :], func=AF.Copy),
                            r=["kT"], w=["kT16.%d" % g2], c=1.5))
                for bi, d in enumerate(BRANCHES):
                    vb = vcount % 2
                    vcount += 1
                    nblk = 48 // d
                    Vv = RS(Vd[vb], nblk, d, 192)
                    if d == 1:
                        S.op("sp", lambda e, vb=vb, pr=pr: e.dma_start(out=Vd[vb], in_=Vs1[pr, :, :]),
                             w=["Vd%d" % vb], dma=("Vd", vb), c=6.0)
                    else:
                        src = Vs2[pr, :, :].rearrange("(blk i r) c -> i blk (r c)", i=128, r=d)
                        dst = RS(Vd[vb], nblk, d * 192)
                        for b0 in range(0, nblk, 6):
                            b1 = min(nblk, b0 + 6)
                            S.op("sp", lambda e, dst=dst, src=src, b0=b0, b1=b1: e.dma_start(out=dst[:, b0:b1, :], in_=src[:, b0:b1, :]),
                                 w=["Vd%d" % vb], dma=("Vd", vb), c=6.0)
                    for qblk in range(16 // d, 48 // d):
                        for r in range(d):
                            if P2_STOP < 2:
                                continue
                            if d == 16:
                                while perm_chunks:
                                    perm_chunks.pop(0)()
                            elif it % 4 == 3 and perm_chunks:
                                perm_chunks.pop(0)()
                            pi = it % 4
                            it += 1
                            ps = RS(banks[pi][:], 4, 128)
                            qi = (it - 1) % 3
                            po = RS(banks[4 + qi][:, 0:256], 2, 128)
                            q0 = qblk * 128 * d + r - NHALO
                            qsl = slice(q0, q0 + 127 * d + 1, d)
                            kp0 = (qblk - 1) * 128 * d + r
                            kc0 = qblk * 128 * d + r
                            ksl = [slice(kp0, kp0 + 127 * d + 1, d), slice(kc0, kc0 + 127 * d + 1, d)]
                            S.op("pe", lambda e, pi=pi: e.matmul(banks[pi][:], lhsT=identb, rhs=mask4, start=True, stop=False),
                                 r=["mask4", "identb"], w=["ps%d" % pi], c=0.25)
                            for hh in range(2):
                                prt = slice(hh * 64, hh * 64 + 64)
                                for kk in range(2):
                                    if d == 16:
                                        n0 = qblk * 128 - NHALO // 16
                                        kb0 = (qblk - 1 + kk) * 128
                                        S.op("pe", lambda e, ps=ps, hh=hh, kk=kk, r=r, n0=n0, kb0=kb0: e.matmul(
                                            ps[:, hh * 2 + kk, :], lhsT=kT16[:, r, kb0:kb0 + 128], rhs=qTz16[:, hh, r, n0:n0 + 128],
                                            start=False, stop=(hh == 1 and kk == 1)),
                                            r=["kT16.%d" % (r // 2), "qT16.%d.%d" % (hh, r // 4)], w=["ps%d" % pi], c=0.06)
                                    else:
                                        S.op("pe", lambda e, ps=ps, hh=hh, kk=kk, ksl=ksl, qsl=qsl: e.matmul(
                                            ps[:, hh * 2 + kk, :], lhsT=kT[:, ksl[kk]], rhs=qTz[:, hh, qsl], start=False, stop=(hh == 1 and kk == 1)),
                                            r=["kT", "qT"], w=["ps%d" % pi], c={1: 0.06, 4: 0.115}[d])
                            P = Pt[pi]
                            S.op("act", lambda e, P=P, ps=ps: e.activation(out=P, in_=ps, func=AF.Exp, scale=0.125),
                                 r=["ps%d" % pi], w=["P%d" % pi], c=0.62)
                            if P2_STOP < 3:
                                continue
                            for hh in range(2):
                                vsl = slice(0, 128) if hh == 0 else slice(64, 192)
                                for kk in range(2):
                                    S.op("pe", lambda e, po=po, hh=hh, kk=kk, vsl=vsl, Vv=Vv, qblk=qblk, r=r, P=P: e.matmul(
                                        po[:, hh, :], lhsT=Vv[:, qblk - 1 + kk, r, vsl], rhs=P[:, hh * 2 + kk, :],
                                        start=(kk == 0), stop=(kk == 1)),
                                        r=["P%d" % pi, "Vd%d" % vb], w=["po%d" % qi], c=0.06)
                            ak = ["acc%d" % gg for gg in range(q0 // 512, (q0 + 127 * d) // 512 + 1)]
                            if bi == 0:
                                S.op("dve", lambda e, po=po, qsl=qsl: e.tensor_copy(out=acc[:, :, qsl], in_=po),
                                     r=["po%d" % qi], w=ak, c=0.45)
                            else:
                                S.op("dve", lambda e, po=po, qsl=qsl: e.tensor_tensor(out=acc[:, :, qsl], in0=acc[:, :, qsl], in1=po, op=ALU.add),
                                     r=["po%d" % qi] + ak, w=ak, c=0.62)
                for tg in range(8 if P2_STOP >= 4 else 0):
                    fi = tg % 2
                    csl = slice(tg * 512, (tg + 1) * 512)
                    S.op("act", lambda e, fi=fi, csl=csl: e.activation(out=sq2[fi], in_=acc[:, :, csl], func=AF.Square),
                         r=["acc%d" % tg], w=["sq2%d" % fi])
                    S.op("pe", lambda e, fi=fi: e.matmul(banks[7][:], lhsT=wreda, rhs=sq2[fi][:, 0, :], start=True, stop=False),
                         r=["sq2%d" % fi, "wreda"], w=["pn"])
                    S.op("pe", lambda e, fi=fi: e.matmul(banks[7][:], lhsT=wredb, rhs=sq2[fi][:, 1, :], start=False, stop=True),
                         r=["sq2%d" % fi, "wredb"], w=["pn"])
                    S.op("act", lambda e: e.activation(out=rs2, in_=banks[7][:], func=AF.Ln), r=["pn"], w=["rs2"], c=0.6)
                    S.op("act", lambda e: e.activation(out=rs2, in_=rs2, func=AF.Exp, scale=-0.5), r=["rs2"], w=["rs2"], c=0.6)
                    S.op("dve", lambda e, fi=fi, csl=csl: e.tensor_tensor(out=ats[fi][0:64, :], in0=acc[0:64, 0, csl], in1=rs2[0:64, :], op=ALU.mult),
                         r=["acc%d" % tg, "rs2"], w=["ats%d" % fi])
                    S.op("dve", lambda e, fi=fi, csl=csl: e.tensor_tensor(out=ats[fi][64:128, :], in0=acc[64:128, 1, csl], in1=rs2[64:128, :], op=ALU.mult),
                         r=["acc%d" % tg, "rs2"], w=["ats%d" % fi])
                    S.op("sp", lambda e, fi=fi, csl=csl, pr=pr: e.dma_start(out=AT[pr, :, csl], in_=ats[fi]),
                         r=["ats%d" % fi], dma=("ats", fi))
            S.barrier()
        A.reset(base_mark)

        if upto >= 3:
            Wout, Wr, gffnb = p3w
            atl = [A.alloc([4, 512], BF16) for _ in range(2)]
            ctl = [A.alloc([4, 512], BF16) for _ in range(2)]
            xt3 = [A.alloc([1024]) for _ in range(2)]
            ht = [A.alloc([1024]) for _ in range(2)]
            xf = [A.alloc([1024]) for _ in range(2)]
            xfT = A.alloc([8, 128])
            xfb_all = A.alloc_top([32, 1024], BF16)
            xfb_keep = A.n
            junk3 = A.alloc([1024])
            ss3 = A.alloc([2]); rstd3 = A.alloc([2])
            for jt in range(32):
                g, s = divmod(jt, 4)
                gi = g % 2
                bi2 = jt % 2
                if s == 0:
                    S.op("sp", lambda e, g=g, gi=gi: e.dma_start(out=atl[gi], in_=AT[:, :, g * 512:(g + 1) * 512].rearrange("a p t -> p a t")),
                         w=["atl%d" % gi], dma=("atl", gi))
                    S.op("sp", lambda e, g=g, gi=gi: e.dma_start(out=ctl[gi], in_=CT[:, :, g * 512:(g + 1) * 512].rearrange("a p t -> p a t")),
                         w=["ctl%d" % gi], dma=("ctl", gi))
                S.op("sp", lambda e, jt=jt, bi2=bi2: e.dma_start(out=xt3[bi2], in_=xin[NHALO + jt * 128:NHALO + (jt + 1) * 128, :]),
                     w=["xt3%d" % bi2], dma=("xt3", bi2))
                ph = banks[2 * bi2:2 * bi2 + 2]
                for half in range(2):
                    for c in range(8):
                        src = atl[gi] if c < 4 else ctl[gi]
                        S.op("pe", lambda e, half=half, c=c, src=src, s=s, ph=ph: e.matmul(
                            ph[half][:], lhsT=src[:, c % 4, s * 128:(s + 1) * 128], rhs=Wout[:, c, half * 512:(half + 1) * 512],
                            start=(c == 0), stop=(c == 7)),
                            r=["atl%d" % gi, "ctl%d" % gi] + pw_keys["Wout"], w=["ph%d%d" % (bi2, half)])
                for half in range(2):
                    hs = slice(half * 512, (half + 1) * 512)
                    S.op("dve", lambda e, half=half, hs=hs, bi2=bi2, ph=ph: e.tensor_tensor(out=ht[bi2][:, hs], in0=xt3[bi2][:, hs], in1=ph[half][:], op=ALU.add),
                         r=["xt3%d" % bi2, "ph%d%d" % (bi2, half)], w=["ht%d" % bi2])
                S.op("sp", lambda e, jt=jt, bi2=bi2: e.dma_start(out=Hd[jt * 128:(jt + 1) * 128, :], in_=ht[bi2]),
                     r=["ht%d" % bi2], dma=("ht", bi2))
                S.op("act", lambda e, bi2=bi2: e.activation(out=junk3, in_=ht[bi2], func=AF.Square, accum_out=ss3[:, 0:1]),
                     r=["ht%d" % bi2], w=["junk3", "ss3"], c=1.05)
                S.op("act", lambda e: e.activation(out=rstd3[:, 0:1], in_=ss3[:, 0:1], func=AF.Sqrt, scale=1.0 / 1024, bias=epst[:, 0:1]),
                     r=["ss3", "epst"], w=["rstd3"])
                S.op("dve", lambda e: e.reciprocal(out=rstd3[:, 0:1], in_=rstd3[:, 0:1]), r=["rstd3"], w=["rstd3"])
                S.op("dve", lambda e, bi2=bi2: e.tensor_scalar_mul(out=xf[bi2], in0=ht[bi2], scalar1=rstd3[:, 0:1]),
                     r=["ht%d" % bi2, "rstd3"], w=["xf%d" % bi2])
                S.op("dve", lambda e, bi2=bi2, jt=jt: e.tensor_tensor(out=xfb_all[:, jt, :], in0=xf[bi2], in1=gffnb, op=ALU.mult),
                     r=["xf%d" % bi2, "gffnb"], w=["xfball%d" % jt], c=0.7)
                pxT = [RS(banks[4][:], 4, 128), RS(banks[5][:], 4, 128)]
                for c in range(8):
                    S.op("pe", lambda e, c=c, bi2=bi2, pxT=pxT: e.matmul(pxT[c // 4][:, c % 4, :], lhsT=xf[bi2][:, c * 128:(c + 1) * 128], rhs=identf, start=True, stop=True),
                         r=["xf%d" % bi2, "identf"], w=["pxT"])
                for hh in range(2):
                    S.op("act", lambda e, hh=hh, pxT=pxT: e.activation(out=xfT[:, hh * 4:(hh + 1) * 4, :], in_=pxT[hh], func=AF.Copy),
                         r=["pxT"], w=["xfT"])
                for c in range(8):
                    S.op("pe", lambda e, c=c: e.matmul(banks[6][:, 0:20], lhsT=xfT[:, c, :], rhs=Wr[:, c, :], start=(c == 0), stop=(c == 7)),
                         r=["xfT", "Wr"], w=["plg"])
                S.op("dve", lambda e, jt=jt: e.tensor_tensor(out=lgall[:, jt, :], in0=banks[6][:, 0:20], in1=rbias, op=ALU.add),
                     r=["plg", "rbias"], w=["lgall"])
            S.barrier()
        A.reset(base_mark)
        A.free_top()
        if upto >= 3:
            A.n = xfb_keep

        if upto >= 4:
            T2 = lambda *d: A.alloc(list(d))
            lg = lgall[:, :, 0:4]
            le4 = lgall[:, :, 4:20]
            mg = T2(32, 1); ohg = T2(32, 4); dg = T2(32, 4); sg = T2(32); pgt = T2(32)
            lsel = T2(32, 4); tmp4 = T2(32, 4)
            m1 = T2(32, 1); oh1 = T2(32, 4); l2 = T2(32, 4); m2 = T2(32, 1); oh2 = T2(32, 4)
            dm = T2(32); ed = T2(32); aa = T2(32)
            oh16 = [T2(32, 16), T2(32, 16)]
            Mf = T2(32, 16); Mb = A.alloc([512], BF16)
            Rk = T2(32, 16); Cn = T2(32, 16); Pj = T2(32, 16)
            ntot = T2(16, 1); cmp17 = T2(16, 17); npad = T2(16); offe = T2(16); endp = T2(16)
            cmpT = T2(NT, 16); eid = T2(NT); widf = T2(NT)
            basep = T2(32, 16); tmp16 = T2(32, 16); posf = T2(2, 32)
            K = "rt"
            dv = lambda fn: S.op("dve", fn, r=[K, "lgall", "thr", "tstart", "pidx"], w=[K])
            fl = lambda t: t.rearrange("p a b -> p (a b)")
            dv(lambda e: e.reduce_max(out=fl(mg), in_=lg, axis=AX.X))
            dv(lambda e: e.tensor_tensor(out=ohg, in0=lg, in1=mg.to_broadcast([128, 32, 4]), op=ALU.is_equal))
            dv(lambda e: e.tensor_tensor(out=dg, in0=lg, in1=mg.to_broadcast([128, 32, 4]), op=ALU.subtract))
            S.op("act", lambda e: e.activation(out=dg, in_=dg, func=AF.Exp), r=[K], w=[K])
            dv(lambda e: e.reduce_sum(out=sg, in_=dg, axis=AX.X))
            dv(lambda e: e.reciprocal(out=pgt, in_=sg))
            for gq_ in range(4):
                dst = lsel if gq_ == 0 else tmp4
                dv(lambda e, gq_=gq_, dst=dst: e.tensor_tensor(out=dst, in0=le4[:, :, gq_ * 4:gq_ * 4 + 4], in1=ohg[:, :, gq_:gq_ + 1].to_broadcast([128, 32, 4]), op=ALU.mult))
                if gq_ > 0:
                    dv(lambda e: e.tensor_tensor(out=lsel, in0=lsel, in1=tmp4, op=ALU.add))
            dv(lambda e: e.reduce_max(out=fl(m1), in_=lsel, axis=AX.X))
            dv(lambda e: e.tensor_tensor(out=oh1, in0=lsel, in1=m1.to_broadcast([128, 32, 4]), op=ALU.is_equal))
            dv(lambda e: e.scalar_tensor_tensor(out=l2, in0=oh1, scalar=-1e30, in1=lsel, op0=ALU.mult, op1=ALU.add))
            dv(lambda e: e.reduce_max(out=fl(m2), in_=l2, axis=AX.X))
            dv(lambda e: e.tensor_tensor(out=oh2, in0=l2, in1=m2.to_broadcast([128, 32, 4]), op=ALU.is_equal))
            dv(lambda e: e.tensor_tensor(out=dm, in0=fl(m2), in1=fl(m1), op=ALU.subtract))
            S.op("act", lambda e: e.activation(out=ed, in_=dm, func=AF.Exp), r=[K], w=[K])
            dv(lambda e: e.tensor_scalar_add(out=aa, in0=ed, scalar1=1.0))
            dv(lambda e: e.reciprocal(out=aa, in_=aa))
            dv(lambda e: e.tensor_tensor(out=gates[:, 0, :], in0=pgt, in1=aa, op=ALU.mult))
            dv(lambda e: e.tensor_tensor(out=ed, in0=ed, in1=aa, op=ALU.mult))
            dv(lambda e: e.tensor_tensor(out=gates[:, 1, :], in0=pgt, in1=ed, op=ALU.mult))
            for sl, ohs in enumerate((oh1, oh2)):
                for gq_ in range(4):
                    dv(lambda e, sl=sl, ohs=ohs, gq_=gq_: e.tensor_tensor(out=oh16[sl][:, :, gq_ * 4:gq_ * 4 + 4], in0=ohs, in1=ohg[:, :, gq_:gq_ + 1].to_broadcast([128, 32, 4]), op=ALU.mult))
            dv(lambda e: e.tensor_tensor(out=Mf, in0=oh16[0], in1=oh16[1], op=ALU.add))
            dv(lambda e: e.tensor_copy(out=Mb, in_=fl(Mf)))
            S.op("pe", lambda e: e.matmul(banks[0][:], lhsT=triu, rhs=Mb, start=True, stop=True), r=[K, "triu"], w=["prk"])
            S.op("pe", lambda e: e.matmul(banks[1][:], lhsT=onesb, rhs=Mb, start=True, stop=True), r=[K, "onesb"], w=["pcn"])
            S.op("dve", lambda e: e.tensor_copy(out=fl(Rk), in_=banks[0][:]), r=["prk", K], w=[K])
            S.op("dve", lambda e: e.tensor_copy(out=fl(Cn), in_=banks[1][:]), r=["pcn", K], w=[K])
            S.op("pool", lambda e: e.memset(fl(Pj), 0.0), r=[K], w=[K])
            S.op("pool", lambda e: e.memset(offe, 0.0), r=[K], w=[K])
            for j in range(1, 32):
                dv(lambda e, j=j: e.tensor_tensor(out=Pj[:, j, :], in0=Pj[:, j - 1, :], in1=Cn[:, j - 1, :], op=ALU.add))
            nt1 = fl(ntot)
            dv(lambda e: e.tensor_tensor(out=nt1, in0=Pj[:, 31, :], in1=Cn[:, 31, :], op=ALU.add))
            dv(lambda e: e.tensor_scalar_add(out=nt1, in0=nt1, scalar1=float(TS - 1)))
            dv(lambda e: e.tensor_tensor(out=cmp17, in0=ntot.to_broadcast([128, 16, 17]), in1=thr[:, None, :].to_broadcast([128, 16, 17]) if False else RS(thr, 1, 17).to_broadcast([128, 16, 17]), op=ALU.is_ge))
            dv(lambda e: e.reduce_sum(out=npad, in_=cmp17, axis=AX.X))
            dv(lambda e: e.tensor_scalar_mul(out=npad, in0=npad, scalar1=float(TS)))
            for ei in range(1, 16):
                dv(lambda e, ei=ei: e.tensor_tensor(out=offe[:, ei:ei + 1], in0=offe[:, ei - 1:ei], in1=npad[:, ei - 1:ei], op=ALU.add))
            dv(lambda e: e.tensor_tensor(out=endp, in0=offe, in1=npad, op=ALU.add))
            dv(lambda e: e.tensor_tensor(out=cmpT, in0=RS(tstart, NT, 1).to_broadcast([128, NT, 16]), in1=RS(endp, 1, 16).to_broadcast([128, NT, 16]), op=ALU.is_ge))
            dv(lambda e: e.reduce_sum(out=eid, in_=cmpT, axis=AX.X))
            dv(lambda e: e.tensor_scalar_min(out=eid, in0=eid, scalar1=15.0))
            dv(lambda e: e.tensor_scalar(out=widf, in0=eid, scalar1=128.0, scalar2=pidx[:, 0:1], op0=ALU.mult, op1=ALU.add))
            dv(lambda e: e.tensor_copy(out=widx_i, in_=widf))
            dv(lambda e: e.tensor_tensor(out=basep, in0=Pj, in1=Rk, op=ALU.add))
            dv(lambda e: e.tensor_tensor(out=basep, in0=basep, in1=RS(offe, 1, 16).to_broadcast([128, 32, 16]), op=ALU.add))
            for sl in range(2):
                dv(lambda e, sl=sl: e.tensor_tensor(out=tmp16, in0=basep, in1=oh16[sl], op=ALU.mult))
                dv(lambda e, sl=sl: e.reduce_sum(out=posf[:, sl, :], in_=tmp16, axis=AX.X))
            dv(lambda e: e.tensor_copy(out=fl(pos_i), in_=fl(posf)))
            if dbg:
                fin.append(S.op("sp", lambda e: e.dma_start(out=dbgo["gates"][:, :], in_=fl(gates)), r=[K], dma=("dbg", 0)))
                fin.append(S.op("sp", lambda e: e.dma_start(out=dbgo["pos"][:, :], in_=fl(pos_i)), r=[K], dma=("dbg", 1)))
                fin.append(S.op("sp", lambda e: e.dma_start(out=dbgo["widx"][:, :], in_=widx_i), r=[K], dma=("dbg", 2)))
                fin.append(S.op("sp", lambda e: e.dma_start(out=dbgo["lg"][:, :], in_=fl(lgall)), r=[K, "lgall"], dma=("dbg", 3)))
            S.barrier()
            zt = A.alloc([4, 1024], BF16)
            S.op("pool", lambda e: e.memset(zt.rearrange("p a b -> p (a b)"), 0.0), w=["zt"])
            for i in range(NT):
                S.op("sp", lambda e, i=i: e.dma_start(out=Xs[i * TS:(i + 1) * TS, :].rearrange("(s p) f -> p s f", p=128), in_=zt),
                     r=["zt"], dma=("zfill",))
            S.barrier()
            for jt in range(32):
                for sl in range(2):
                    S.op("pool", lambda e, jt=jt, sl=sl: e.indirect_dma_start(
                        out=Xs[:, :], out_offset=bass.IndirectOffsetOnAxis(ap=pos_i[:, sl, jt:jt + 1], axis=0),
                        in_=xfb_all[:, jt, :], in_offset=None),
                        r=["xfball%d" % jt], w=["scq%d" % ((2 * jt + sl) % 6)], dma=("xscat", (2 * jt + sl) % 6))
            S.barrier()
            A.free_top()
        A.reset(base_mark)
        rt_mark = A.mark()

        if upto >= 5:
            p6w = prep_p6() if upto >= 6 else None
            W1g = [A.alloc([8, 512], BF16) for _ in range(2)]
            W3g = [A.alloc([8, 512], BF16) for _ in range(2)]
            W2g = [A.alloc([4, 1024], BF16) for _ in range(2)]
            Xt = [A.alloc([4, 1024], BF16) for _ in range(2)]
            XTt = [A.alloc([8, 512], BF16) for _ in range(2)]
            s1 = [A.alloc([512]) for _ in range(2)]
            GT = [A.alloc([4, 512], BF16) for _ in range(2)]
            Yt = [A.alloc([4, 1024]) for _ in range(2)]
            for i in range(NT):
                b = i % 2
                fl3 = lambda t: t.rearrange("p a b -> p (a b)")
                for wi, (wt, wd) in enumerate(((W1g, w1), (W3g, w3), (W2g, w2))):
                    S.op("pool", lambda e, wt=wt, wd=wd, b=b, i=i: e.indirect_dma_start(
                        out=fl3(wt[b]), out_offset=None, in_=wd[:, :],
                        in_offset=bass.IndirectOffsetOnAxis(ap=widx_i[:, i:i + 1], axis=0)),
                        w=["W%d_%d" % (wi, b)], dma=("wg", wi, b))
                S.op("sp", lambda e, b=b, i=i: e.dma_start(out=Xt[b], in_=Xs[i * TS:(i + 1) * TS, :].rearrange("(s p) f -> p s f", p=128)),
                     w=["Xt%d" % b], dma=("Xt", b))
                for s in range(4):
                    pb_ = 6 + (s % 2)
                    ptx = RS(PB(pb_), 8, 128)
                    for c in range(8):
                        S.op("pe", lambda e, s=s, c=c, b=b, ptx=ptx: e.transpose(out=ptx[:, c, :], in_=Xt[b][:, s, c:1024:8], identity=identb),
                             r=["Xt%d" % b, "identb"], w=["ptx%d" % (s % 2)], c=0.11)
                    eng = "act" if s % 2 == 0 else "dve"
                    if eng == "act":
                        S.op("act", lambda e, s=s, b=b, ptx=ptx: e.activation(out=XTt[b][:, :, s * 128:(s + 1) * 128], in_=ptx, func=AF.Copy),
                             r=["ptx%d" % (s % 2)], w=["XT%d" % b])
                    else:
                        S.op("dve", lambda e, s=s, b=b, ptx=ptx: e.tensor_copy(out=XTt[b][:, :, s * 128:(s + 1) * 128], in_=ptx),
                             r=["ptx%d" % (s % 2)], w=["XT%d" % b])
                for c4 in range(4):
                    hb = c4 % 2
                    for wi, wt in enumerate((W1g, W3g)):
                        bk = banks[hb * 2 + wi]
                        for k in range(8):
                            S.op("pe", lambda e, bk=bk, wt=wt, k=k, c4=c4, b=b: e.matmul(
                                bk[:], lhsT=wt[b][:, k, c4:512:4], rhs=XTt[b][:, k, :], start=(k == 0), stop=(k == 7)),
                                r=["XT%d" % b, "W%d_%d" % (wi, b)], w=["phh%d%d" % (hb, wi)])
                    S.op("act", lambda e, hb=hb: e.activation(out=s1[hb], in_=banks[hb * 2][:], func=AF.Silu),
                         r=["phh%d0" % hb], w=["s1%d" % hb])
                    S.op("dve", lambda e, hb=hb, c4=c4, b=b: e.tensor_tensor(out=GT[b][:, c4, :], in0=s1[hb], in1=banks[hb * 2 + 1][:], op=ALU.mult),
                         r=["s1%d" % hb, "phh%d1" % hb], w=["GT%d" % b])
                for s in range(4):
                    for half in range(2):
                        yb = 4 + half
                        for c4 in range(4):
                            S.op("pe", lambda e, yb=yb, s=s, half=half, c4=c4, b=b: e.matmul(
                                banks[yb][:], lhsT=GT[b][:, c4, s * 128:(s + 1) * 128], rhs=W2g[b][:, c4, half * 512:(half + 1) * 512],
                                start=(c4 == 0), stop=(c4 == 3)),
                                r=["GT%d" % b, "W2_%d" % b], w=["py%d" % half])
                        if half == 0:
                            S.op("act", lambda e, yb=yb, s=s, b=b: e.activation(out=Yt[b][:, s, 0:512], in_=banks[yb][:], func=AF.Copy),
                                 r=["py0"], w=["Yt%d" % b])
                        else:
                            S.op("dve", lambda e, yb=yb, s=s, b=b: e.tensor_copy(out=Yt[b][:, s, 512:1024], in_=banks[yb][:]),
                                 r=["py1"], w=["Yt%d" % b])
                S.op("sp", lambda e, b=b, i=i: e.dma_start(out=Ys[i * TS:(i + 1) * TS, :].rearrange("(s p) f -> p s f", p=128), in_=Yt[b]),
                     r=["Yt%d" % b], dma=("Yt", b))
            S.barrier()
        A.reset(rt_mark)

        if upto >= 6:
            Wg, Wp, gpostb = p6w
            NB6 = 3
            h5 = [A.alloc([1024]) for _ in range(NB6)]
            y0 = [A.alloc([1024]) for _ in range(NB6)]
            y1 = [A.alloc([1024]) for _ in range(NB6)]
            xpb = [A.alloc([1024], BF16) for _ in range(NB6)]
            xpT = [A.alloc([8, 128], BF16) for _ in range(NB6)]
            pt5 = [A.alloc([256]) for _ in range(NB6)]
            pb5 = [A.alloc([256], BF16) for _ in range(NB6)]
            ppT = [A.alloc([2, 128], BF16) for _ in range(NB6)]
            gt5 = [A.alloc([1024]) for _ in range(NB6)]
            pr5 = [A.alloc([1024]) for _ in range(NB6)]
            junk5 = A.alloc([1024])
            ss5 = A.alloc([2]); rstd5 = A.alloc([2]); ss6 = A.alloc([2]); rstd6 = A.alloc([2])
            for jt in range(32):
                b = jt % NB6
                rows = slice(jt * 128, (jt + 1) * 128)
                S.op("sp", lambda e, b=b, rows=rows: e.dma_start(out=h5[b], in_=Hd[rows, :]), w=["h5%d" % b], dma=("h5", b))
                S.op("sp", lambda e, b=b, rows=rows: e.dma_start(out=pt5[b], in_=pin[rows, :]), w=["pt5%d" % b], dma=("pt5", b))
                for sl, yy in enumerate((y0, y1)):
                    S.op("pool", lambda e, sl=sl, yy=yy, b=b, jt=jt: e.indirect_dma_start(
                        out=yy[b], out_offset=None, in_=Ys[:, :],
                        in_offset=bass.IndirectOffsetOnAxis(ap=pos_i[:, sl, jt:jt + 1], axis=0)),
                        w=["y%d%d" % (sl, b)], dma=("yg", sl, b))
                S.op("dve", lambda e, b=b, jt=jt: e.scalar_tensor_tensor(out=h5[b], in0=y0[b], scalar=gates[:, 0, jt:jt + 1], in1=h5[b], op0=ALU.mult, op1=ALU.add),
                     r=["y0%d" % b, "h5%d" % b], w=["h5%d" % b])
                S.op("dve", lambda e, b=b, jt=jt: e.scalar_tensor_tensor(out=h5[b], in0=y1[b], scalar=gates[:, 1, jt:jt + 1], in1=h5[b], op0=ALU.mult, op1=ALU.add),
                     r=["y1%d" % b, "h5%d" % b], w=["h5%d" % b])
                S.op("act", lambda e, b=b: e.activation(out=junk5, in_=h5[b], func=AF.Square, accum_out=ss5[:, 0:1]),
                     r=["h5%d" % b], w=["junk5", "ss5"], c=1.05)
                S.op("act", lambda e: e.activation(out=rstd5[:, 0:1], in_=ss5[:, 0:1], func=AF.Sqrt, scale=1.0 / 1024, bias=epst[:, 0:1]),
                     r=["ss5", "epst"], w=["rstd5"])
                S.op("dve", lambda e: e.reciprocal(out=rstd5[:, 0:1], in_=rstd5[:, 0:1]), r=["rstd5"], w=["rstd5"])
                S.op("dve", lambda e, b=b: e.tensor_scalar_mul(out=xpb[b], in0=h5[b], scalar1=rstd5[:, 0:1]),
                     r=["h5%d" % b, "rstd5"], w=["xpb%d" % b], c=0.7)
                ptr5 = RS(PB(6), 8, 128)
                for c in range(8):
                    S.op("pe", lambda e, c=c, b=b: e.transpose(out=ptr5[:, c, :], in_=xpb[b][:, c * 128:(c + 1) * 128], identity=identb),
                         r=["xpb%d" % b, "identb"], w=["ptr5"], c=0.11)
                S.op("act", lambda e, b=b: e.activation(out=xpT[b], in_=ptr5, func=AF.Copy), r=["ptr5"], w=["xpT%d" % b])
                S.op("act", lambda e, b=b: e.activation(out=pb5[b], in_=pt5[b], func=AF.Copy), r=["pt5%d" % b], w=["pb5%d" % b])
                ptp = RS(PB(7)[:, 0:256], 2, 128)
                for c in range(2):
                    S.op("pe", lambda e, c=c, b=b: e.transpose(out=ptp[:, c, :], in_=pb5[b][:, c * 128:(c + 1) * 128], identity=identb),
                         r=["pb5%d" % b, "identb"], w=["ptp"], c=0.11)
                S.op("dve", lambda e, b=b: e.tensor_copy(out=ppT[b], in_=ptp), r=["ptp"], w=["ppT%d" % b])
                pg_ = banks[0:2] if jt % 2 == 0 else banks[2:4]
                for half in range(2):
                    for c in range(8):
                        S.op("pe", lambda e, half=half, c=c, b=b, pg_=pg_: e.matmul(
                            pg_[half][:], lhsT=xpT[b][:, c, :], rhs=Wg[:, c, half * 512:(half + 1) * 512], start=(c == 0), stop=(c == 7)),
                            r=["xpT%d" % b] + pw_keys["Wg"], w=["pg%d%d" % (jt % 2, half)])
                    S.op("act", lambda e, half=half, b=b, pg_=pg_: e.activation(out=gt5[b][:, half * 512:(half + 1) * 512], in_=pg_[half][:], func=AF.Sigmoid),
                         r=["pg%d%d" % (jt % 2, half)], w=["gt5%d" % b])
                for half in range(2):
                    for c in range(2):
                        S.op("pe", lambda e, half=half, c=c, b=b: e.matmul(
                            banks[4 + half][:], lhsT=ppT[b][:, c, :], rhs=Wp[:, c, half * 512:(half + 1) * 512], start=(c == 0), stop=(c == 1)),
                            r=["ppT%d" % b] + pw_keys["Wp"], w=["pp%d" % half])
                    S.op("act", lambda e, half=half: e.activation(out=junk5[:, 0:512], in_=banks[4 + half][:], func=AF.Square, accum_out=ss6[:, half:half + 1]),
                         r=["pp%d" % half], w=["junk5", "ss6"])
                S.op("dve", lambda e: e.tensor_tensor(out=ss6[:, 0:1], in0=ss6[:, 0:1], in1=ss6[:, 1:2], op=ALU.add), r=["ss6"], w=["ss6"])
                S.op("act", lambda e: e.activation(out=rstd6[:, 0:1], in_=ss6[:, 0:1], func=AF.Sqrt, scale=1.0 / 1024, bias=epst[:, 0:1]),
                     r=["ss6", "epst"], w=["rstd6"])
                S.op("dve", lambda e: e.reciprocal(out=rstd6[:, 0:1], in_=rstd6[:, 0:1]), r=["rstd6"], w=["rstd6"])
                for half in range(2):
                    hs = slice(half * 512, (half + 1) * 512)
                    S.op("dve", lambda e, half=half, hs=hs, b=b: e.scalar_tensor_tensor(out=pr5[b][:, hs], in0=banks[4 + half][:], scalar=rstd6[:, 0:1], in1=gpostb[:, hs], op0=ALU.mult, op1=ALU.mult),
                         r=["pp%d" % half, "rstd6", "gpostb"], w=["pr5%d" % b])
                S.op("dve", lambda e, b=b: e.tensor_tensor(out=pr5[b], in0=pr5[b], in1=gt5[b], op=ALU.mult),
                     r=["pr5%d" % b, "gt5%d" % b], w=["pr5%d" % b], c=0.7)
                S.op("dve", lambda e, b=b: e.tensor_tensor(out=pr5[b], in0=pr5[b], in1=h5[b], op=ALU.add),
                     r=["pr5%d" % b, "h5%d" % b], w=["pr5%d" % b], c=0.7)
                fin.append(S.op("sp", lambda e, b=b, rows=rows: e.dma_start(out=out[rows, :], in_=pr5[b]),
                                r=["pr5%d" % b], dma=("out", b)))
        else:
            z = A.alloc([1024])
            S.op("pool", lambda e: e.memset(z, 0.0), w=["z"])
            fin.append(S.op("sp", lambda e: e.dma_start(out=out[0:128, :], in_=z), r=["z"], dma=("out", 0)))
        S.emit(final_wait_ops=fin)
    return nc


def _consts():
    k = np.arange(128)[:, None]
    q = np.arange(128)[None, :]
    prev = (q <= k).astype(np.float32)
    cur = (q >= k).astype(np.float32)
    mask4 = (np.concatenate([prev, cur, prev, cur], axis=1) - 1.0) * 240000.0
    ident = np.eye(128, dtype=np.float32)
    blk64 = np.zeros((128, 128), np.float32)
    blk64[:64, :64] = 1.0 / 64
    blk64[64:, 64:] = 1.0 / 64
    wa = np.zeros((128, 128), np.float32)
    wa[:64, :64] = 1.0 / 64
    wa[64:, :64] = EPS / 64
    wb = np.zeros((128, 128), np.float32)
    wb[64:, 64:] = 1.0 / 64
    wb[:64, 64:] = EPS / 64
    triu = (k < q).astype(np.float32)
    thr = np.broadcast_to((np.arange(17, dtype=np.float32) + 1) * TS, (128, 17)).copy()
    tstart = np.broadcast_to(np.arange(NT, dtype=np.float32) * TS, (128, NT)).copy()
    pidx = np.arange(128, dtype=np.float32).reshape(128, 1)
    return dict(ident=ident, mask4=mask4, blk64=blk64, wreda=wa, wredb=wb, triu=triu, thr=thr, tstart=tstart, pidx=pidx)


def _pc(v, nch):
    return np.ascontiguousarray(np.asarray(v, np.float32).reshape(nch, 128).T)


def _bc(v):
    v = np.asarray(v, np.float32).reshape(1, -1)
    return np.ascontiguousarray(np.broadcast_to(v, (128, v.shape[1])))


def make_in_maps(x, p, g_mix, w_in, q_norm, k_norm, conv_w, g_attn_out, g_conv_out, w_out, g_ffn,
                 w_router_group, b_router_group, w_router_expert, b_router_expert, w1, w3, w2,
                 g_ple, w_ple_gate, w_ple_proj, g_ple_post):
    f = lambda a: np.ascontiguousarray(np.asarray(a, np.float32))
    x = f(x); p = f(p)
    cs = _consts()
    shared = dict(cs)
    shared["gmix"] = _pc(g_mix[0], 8)
    shared["gq_b"] = _bc(np.tile(f(q_norm[0]), 8))
    shared["gk_b"] = _bc(np.tile(f(k_norm[0]), 8))
    shared["cw"] = np.ascontiguousarray(f(conv_w[0]).reshape(3, 4, 128).transpose(2, 1, 0).reshape(128, 12))
    shared["gout_pc"] = _pc(np.concatenate([f(g_attn_out[0]), f(g_conv_out[0])]), 8)
    shared["gffn_b"] = _bc(g_ffn[0])
    shared["gffn_pc"] = _pc(g_ffn[0], 8)
    shared["gple_pc"] = _pc(g_ple[0], 8)
    shared["gpost_b"] = _bc(g_ple_post[0])
    shared["rbias_b"] = _bc(np.concatenate([f(b_router_group[0]), f(b_router_expert[0])]))
    shared["w_in"] = f(w_in[0])
    shared["w_out"] = f(w_out[0])
    shared["w_r"] = np.ascontiguousarray(np.concatenate([f(w_router_group[0]), f(w_router_expert[0])], axis=1))
    shared["w1"] = f(w1[0]).reshape(16 * 128, 8 * 512)
    shared["w3"] = f(w3[0]).reshape(16 * 128, 8 * 512)
    shared["w2"] = f(w2[0]).reshape(16 * 128, 4 * 1024)
    shared["w_pg"] = f(w_ple_gate[0])
    shared["w_pp"] = f(w_ple_proj[0])
    half_inv = 500000.0 ** (-np.arange(0, 16, 2, dtype=np.float64) / 16)
    maps = []
    for c in range(8):
        b, hf = divmod(c, 2)
        t0 = hf * NTOK - NHALO
        xin = np.zeros((NALL, 1024), np.float32)
        lo = max(t0, 0)
        xin[lo - t0:, :] = x[b, lo:t0 + NALL, :]
        tau = np.arange(NALL)
        exists = ((tau + t0) >= 0).astype(np.float32)
        ang = (tau + t0).astype(np.float64)[:, None] * half_inv[None, :]
        lay = lambda a: np.ascontiguousarray(a.reshape(48, 128, -1).transpose(1, 0, 2).reshape(128, -1).astype(np.float32))
        m = dict(shared)
        m["xin"] = xin
        m["pin"] = np.ascontiguousarray(p[0, b, hf * NTOK:(hf + 1) * NTOK, :])
        m["ex"] = lay(exists[:, None])
        m["cos"] = lay(np.cos(ang))
        m["sin"] = lay(np.sin(ang))
        maps.append(m)
    return maps


_NC_CACHE = {}


def kernel(**inputs):
    if "nc" not in _NC_CACHE:
        _NC_CACHE["nc"] = build_nc()
    nc = _NC_CACHE["nc"]
    maps = make_in_maps(**inputs)
    res = run_bass_kernel_spmd(nc, maps, core_ids=list(range(8)))
    outs = [np.asarray(r["out"], np.float32) for r in res.results]
    full = np.zeros((4, 8192, 1024), np.float32)
    for c in range(8):
        b, hf = divmod(c, 2)
        full[b, hf * NTOK:(hf + 1) * NTOK, :] = outs[c]
    return full
```
